# Optimizing a Trainium2 kernel written in Bass

```python
import math
import jax, jax.numpy as jnp
from jax import lax
import numpy as np

D_MODEL = 1024
BATCH = 2
SEQ = 8192
DEPTH = 2

CHUNK = 64
N_A = DEPTH // 2
N_B = DEPTH - N_A
EPS = 1e-6

POOL_WINDOWS = (2, 4, 8, 16)
N_POOL = len(POOL_WINDOWS)
POOL_GD = D_MODEL // N_POOL

N_HEADS = 8
QK_NOPE = 128
QK_ROPE = 64
V_DIM = 128
Q_RANK = 384
KV_RANK = 256
ROPE_THETA = 10000.0
ATTN_SCALE = 1.0 / math.sqrt(QK_NOPE + QK_ROPE)
Q_BLOCK = 128

N_GROUPS = 4
EXPERTS_PER_GROUP = 8
N_EXPERTS = N_GROUPS * EXPERTS_PER_GROUP
TOP_K = 2
D_EXPERT = 512
ROW_BLOCK = 256

kernel_name = "yoco_pool_mla_hier_moe"


def rms_norm(x, g):
    xf = x.astype(jnp.float32)
    y = xf * lax.rsqrt(jnp.mean(xf * xf, axis=-1, keepdims=True) + EPS)
    return (y * g.astype(jnp.float32)).astype(x.dtype)


def rope_tables(seq, dtype):
    half = QK_ROPE // 2
    inv = ROPE_THETA ** (-jnp.arange(half, dtype=jnp.float32) / half)
    ang = jnp.arange(seq, dtype=jnp.float32)[:, None] * inv[None, :]
    return jnp.cos(ang).astype(dtype), jnp.sin(ang).astype(dtype)


def apply_rope(x, cos, sin):
    half = QK_ROPE // 2
    x1, x2 = x[..., :half], x[..., half:]
    return jnp.concatenate([x1 * cos - x2 * sin, x2 * cos + x1 * sin], axis=-1)


def pool_mixer(h, w, b, scale):
    B, S, D = h.shape
    hg = h.reshape(B, S, N_POOL, POOL_GD).astype(jnp.float32)
    csum = jnp.pad(jnp.cumsum(hg, axis=1), ((0, 0), (1, 0), (0, 0), (0, 0)))
    t = jnp.arange(S)
    means = []
    for g, win in enumerate(POOL_WINDOWS):
        lo = jnp.maximum(t + 1 - win, 0)
        cnt = (t + 1 - lo).astype(jnp.float32)
        means.append((csum[:, 1:, g] - csum[:, lo, g]) / cnt[:, None])
    pooled = (jnp.stack(means, axis=2) - hg).astype(h.dtype)
    y = jnp.einsum('bsgc,gcd->bsgd', pooled, w) + b
    return y.reshape(B, S, D) * scale


def shared_kv(x, kv_in_norm, w_dkv, kv_norm, w_uk, w_uv, cos, sin):
    B, S, _ = x.shape
    hs = rms_norm(x, kv_in_norm)
    ckv = hs @ w_dkv
    c_kv = rms_norm(ckv[..., :KV_RANK], kv_norm)
    k_rope = apply_rope(ckv[..., KV_RANK:], cos, sin)
    k_nope = (c_kv @ w_uk).reshape(B, S, N_HEADS, QK_NOPE)
    v = (c_kv @ w_uv).reshape(B, S, N_HEADS, V_DIM)
    return k_nope, k_rope, v


def mla_attention(h, k_nope, k_rope, v, wq_down, q_norm, wq_up, wo, cos, sin):
    B, S, _ = h.shape
    cq = rms_norm(h @ wq_down, q_norm)
    q = (cq @ wq_up).reshape(B, S, N_HEADS, QK_NOPE + QK_ROPE)
    q_nope = q[..., :QK_NOPE]
    q_rope = apply_rope(q[..., QK_NOPE:], cos[:, None, :], sin[:, None, :])
    nb = S // Q_BLOCK
    qn_b = q_nope.reshape(B, nb, Q_BLOCK, N_HEADS, QK_NOPE).transpose(1, 0, 2, 3, 4)
    qr_b = q_rope.reshape(B, nb, Q_BLOCK, N_HEADS, QK_ROPE).transpose(1, 0, 2, 3, 4)
    k_chunk = jnp.arange(S) // CHUNK

    def attend(args):
        qn, qr, i = args
        s = (jnp.einsum('bqhd,bkhd->bhqk', qn, k_nope)
             + jnp.einsum('bqhr,bkr->bhqk', qr, k_rope)).astype(jnp.float32) * ATTN_SCALE
        q_chunk = (i * Q_BLOCK + jnp.arange(Q_BLOCK)) // CHUNK
        mask = k_chunk[None, :] <= q_chunk[:, None]
        s = jnp.where(mask[None, None], s, -jnp.inf)
        p = jax.nn.softmax(s, axis=-1).astype(v.dtype)
        return jnp.einsum('bhqk,bkhd->bqhd', p, v)

    o = lax.map(attend, (qn_b, qr_b, jnp.arange(nb)))
    o = o.transpose(1, 0, 2, 3, 4).reshape(B, S, N_HEADS * V_DIM)
    return o @ wo


def hier_moe(h, rg_w, rg_b, re_w, re_b, w_gate, w_up, w_down):
    B, S, D = h.shape
    T = B * S
    ht = h.reshape(T, D)
    gp = jax.nn.softmax((ht @ rg_w + rg_b).astype(jnp.float32), axis=-1)
    g_idx = jnp.argmax(gp, axis=-1)
    g_prob = jnp.take_along_axis(gp, g_idx[:, None], axis=-1)[:, 0]
    el = (ht @ re_w + re_b).astype(jnp.float32).reshape(T, N_GROUPS, EXPERTS_PER_GROUP)
    el = jnp.take_along_axis(el, g_idx[:, None, None], axis=1)[:, 0]
    top_p, top_i = lax.top_k(jax.nn.softmax(el, axis=-1), TOP_K)
    gates = g_prob[:, None] * top_p / jnp.sum(top_p, axis=-1, keepdims=True)
    expert_ids = (g_idx[:, None] * EXPERTS_PER_GROUP + top_i).astype(jnp.int32)

    N = T * TOP_K
    flat_e = expert_ids.reshape(N)
    flat_tok = jnp.arange(N, dtype=jnp.int32) // TOP_K
    flat_g = gates.reshape(N)
    order = jnp.argsort(flat_e, stable=True)
    sorted_e = flat_e[order]
    counts = jnp.bincount(flat_e, length=N_EXPERTS).astype(jnp.int32)
    padded = ((counts + ROW_BLOCK - 1) // ROW_BLOCK) * ROW_BLOCK
    pad_end = jnp.cumsum(padded)
    pad_start = pad_end - padded
    start = jnp.cumsum(counts) - counts
    dest = pad_start[sorted_e] + (jnp.arange(N, dtype=jnp.int32) - start[sorted_e])
    n_blocks = N // ROW_BLOCK + N_EXPERTS
    n_rows = n_blocks * ROW_BLOCK
    row_tok = jnp.zeros((n_rows,), jnp.int32).at[dest].set(flat_tok[order])
    row_w = jnp.zeros((n_rows,), jnp.float32).at[dest].set(flat_g[order])
    block_e = jnp.clip(jnp.searchsorted(pad_end, jnp.arange(n_blocks) * ROW_BLOCK, side='right'),
                       0, N_EXPERTS - 1).astype(jnp.int32)
    xb = ht[row_tok].reshape(n_blocks, ROW_BLOCK, D)

    def expert_block(args):
        xr, e = args
        return (jax.nn.silu(xr @ w_gate[e]) * (xr @ w_up[e])) @ w_down[e]

    y = lax.map(expert_block, (xb, block_e)).reshape(n_rows, D).astype(jnp.float32)
    out = jnp.zeros((T, D), jnp.float32).at[row_tok].add(y * row_w[:, None])
    return out.astype(h.dtype).reshape(B, S, D)


def setup_inputs(seed: int = 0) -> dict:
    key = jax.random.key(seed)
    ks = iter(jax.random.split(key, 32))
    f32 = jnp.float32

    def nrm(shape, fan_in):
        return jax.random.normal(next(ks), shape, f32) * (fan_in ** -0.5)

    def gain(shape):
        return 1.0 + 0.02 * jax.random.normal(next(ks), shape, f32)

    def small(shape, s=0.01):
        return s * jax.random.normal(next(ks), shape, f32)

    return {
        "x": jax.random.normal(next(ks), (BATCH, SEQ, D_MODEL), f32),
        "pool_norm": gain((N_A, D_MODEL)),
        "pool_w": nrm((N_A, N_POOL, POOL_GD, POOL_GD), POOL_GD),
        "pool_b": small((N_A, N_POOL, POOL_GD)),
        "pool_scale": gain((N_A, D_MODEL)),
        "kv_in_norm": gain((D_MODEL,)),
        "w_dkv": nrm((D_MODEL, KV_RANK + QK_ROPE), D_MODEL),
        "kv_norm": gain((KV_RANK,)),
        "w_uk": nrm((KV_RANK, N_HEADS * QK_NOPE), KV_RANK),
        "w_uv": nrm((KV_RANK, N_HEADS * V_DIM), KV_RANK),
        "attn_norm": gain((N_B, D_MODEL)),
        "wq_down": nrm((N_B, D_MODEL, Q_RANK), D_MODEL),
        "q_norm": gain((N_B, Q_RANK)),
        "wq_up": nrm((N_B, Q_RANK, N_HEADS * (QK_NOPE + QK_ROPE)), Q_RANK),
        "wo": nrm((N_B, N_HEADS * V_DIM, D_MODEL), N_HEADS * V_DIM),
        "ffn_norm": gain((DEPTH, D_MODEL)),
        "router_group_w": nrm((DEPTH, D_MODEL, N_GROUPS), D_MODEL),
        "router_group_b": small((DEPTH, N_GROUPS)),
        "router_expert_w": nrm((DEPTH, D_MODEL, N_EXPERTS), D_MODEL),
        "router_expert_b": small((DEPTH, N_EXPERTS)),
        "w_gate": nrm((DEPTH, N_EXPERTS, D_MODEL, D_EXPERT), D_MODEL),
        "w_up": nrm((DEPTH, N_EXPERTS, D_MODEL, D_EXPERT), D_MODEL),
        "w_down": nrm((DEPTH, N_EXPERTS, D_EXPERT, D_MODEL), D_EXPERT),
        "final_norm": gain((D_MODEL,)),
    }


def reference(x, pool_norm, pool_w, pool_b, pool_scale, kv_in_norm, w_dkv, kv_norm, w_uk, w_uv,
              attn_norm, wq_down, q_norm, wq_up, wo, ffn_norm, router_group_w, router_group_b,
              router_expert_w, router_expert_b, w_gate, w_up, w_down, final_norm):
    S = x.shape[1]
    cos, sin = rope_tables(S, x.dtype)
    k_nope = k_rope = v = None
    for l in range(DEPTH):
        if l < N_A:
            x = x + pool_mixer(rms_norm(x, pool_norm[l]), pool_w[l], pool_b[l], pool_scale[l])
        else:
            if l == N_A:
                k_nope, k_rope, v = shared_kv(x, kv_in_norm, w_dkv, kv_norm, w_uk, w_uv, cos, sin)
            j = l - N_A
            x = x + mla_attention(rms_norm(x, attn_norm[j]), k_nope, k_rope, v,
                                  wq_down[j], q_norm[j], wq_up[j], wo[j], cos, sin)
        x = x + hier_moe(rms_norm(x, ffn_norm[l]), router_group_w[l], router_group_b[l],
                         router_expert_w[l], router_expert_b[l], w_gate[l], w_up[l], w_down[l])
    return rms_norm(x, final_norm)
```

```python
import numpy as np
import ml_dtypes
import concourse.bass as bass
import concourse.mybir as mybir
from concourse.bass_utils import run_bass_kernel_spmd

F32 = mybir.dt.float32
BF16 = mybir.dt.bfloat16
I32 = mybir.dt.int32
ALU = mybir.AluOpType
AF = mybir.ActivationFunctionType
AX = mybir.AxisListType
NPBF = ml_dtypes.bfloat16

NCORES = 8
D = 1024
SEQ = 8192
TPC = 2048
NT = TPC // 128
E = 32
CAP = 256
NSLOT = E * CAP
EPS = 1e-6
ATTN_SCALE = 1.0 / float(np.sqrt(192.0))
BIG = 1.0e4


class Tn:
    def __init__(self, h, name):
        self.h = h
        self.name = name
        self.w = []
        self.r = []

    def __getitem__(self, k):
        return self.h[k]


class Prog:
    ENG = ("pe", "act", "dve", "pool", "sp")

    def __init__(self, nc):
        self.nc = nc
        self.q = {k: [] for k in self.ENG}
        self.esem = {k: nc.alloc_semaphore("es_" + k) for k in ("pe", "act", "dve", "pool")}
        self.ecnt = {k: 0 for k in self.esem}
        self.seen = {k: {} for k in self.ENG}
        self.lanes = {}
        self.free_lanes = []
        self.nlane = 0
        self.final = []
        self.phase = 0
        self._sb_off = 16512
        self._sb_top = 229344
        self._shared = {}

    def sb(self, name, shape, dt):
        nb = 1
        for d_ in shape[1:]:
            nb *= d_
        nb *= {F32: 4, BF16: 2, I32: 4}[dt]
        off = (self._sb_off + 63) // 64 * 64
        assert off + nb <= self._sb_top, f"SBUF arena overflow at {name}: {off}+{nb}"
        h = self.nc.alloc_sbuf_tensor_at(f"s{self.phase}_" + name, list(shape), dt, offset=off)
        self._sb_off = off + nb
        return Tn(h, name)

    def mark(self):
        return self._sb_off

    def release(self, mark):
        self.barrier()
        self._sb_off = mark
        self._shared = {}
        self.phase += 1

    def barrier(self):
        toks = [(self.esem[k], "es_" + k, self.ecnt[k]) for k in self.esem]
        toks += [(L[0], L[2], L[1]) for L in self.lanes.values()]
        for eng in self.ENG:
            seen = self.seen[eng]
            waits = []
            for (sem, key, val) in toks:
                if val <= 0 or seen.get(key, -1) >= val:
                    continue
                seen[key] = val
                waits.append((sem, val))

            def emit(e, waits=waits):
                for (s_, v) in waits:
                    e.wait_ge(s_, v)
            self.q[eng].append(emit)
        self.free_lanes += [L for L in self.lanes.values() if L[3] != "pool" and L[1] % 16 == 0]
        self.lanes = {}

    def bc_reg(self, e):
        if getattr(self, "_bc", None) is None:
            self._bc = e.to_reg(NSLOT - 1)
        return self._bc

    def shared(self, name, shape, dt):
        if name not in self._shared:
            self._shared[name] = (self.sb(name, shape, dt), list(shape), dt)
        t, sh, d = self._shared[name]
        assert sh == list(shape) and d == dt, name
        return t

    def psum(self, name, shape, dt=F32):
        return Tn(self.nc.alloc_psum_tensor("p_" + name, list(shape), dt), name)

    def dram(self, name, shape, dt, kind="Internal"):
        return Tn(self.nc.dram_tensor(f"d{self.phase}_" + name, list(shape), dt, kind=kind), name)

    def _waits(self, eng, r, w):
        toks = []
        for b in r:
            toks += b.w
        for b in w:
            toks += b.w
            toks += b.r
        out = []
        seen = self.seen[eng]
        for (sem, key, val) in toks:
            if eng == "pe" and key == "es_pe":
                continue
            if seen.get(key, -1) >= val:
                continue
            seen[key] = val
            out.append((sem, val))
        return out

    def _commit(self, tok, r, w):
        for b in r:
            b.r.append(tok)
        for b in w:
            b.w = [tok]
            b.r = []

    def op(self, eng, fn, r=(), w=()):
        waits = self._waits(eng, r, w)
        self.ecnt[eng] += 1
        sem = self.esem[eng]
        tok = (sem, "es_" + eng, self.ecnt[eng])

        def emit(e, waits=waits, fn=fn, sem=sem):
            for (s, v) in waits:
                e.wait_ge(s, v)
            fn(e).then_inc(sem, 1)

        self.q[eng].append(emit)
        self._commit(tok, r, w)
        return tok

    def dma(self, q, fn, r=(), w=(), lane=None, final=False, inc=16):
        waits = self._waits(q, r, w)
        if lane not in self.lanes:
            if q != "pool" and self.free_lanes:
                self.lanes[lane] = self.free_lanes.pop()
            else:
                self.nlane += 1
                self.lanes[lane] = [self.nc.alloc_semaphore(f"ln{self.nlane}"), 0, f"ln{self.nlane}", q]
        assert self.lanes[lane][3] == q, f"lane {lane} used from two queues"
        L = self.lanes[lane]
        L[1] += inc
        sem = L[0]
        tok = (sem, L[2], L[1])

        def emit(e, waits=waits, fn=fn, sem=sem, inc=inc):
            for (s, v) in waits:
                e.wait_ge(s, v)
            fn(e).then_inc(sem, inc)

        self.q[q].append(emit)
        self._commit(tok, r, w)
        if final:
            self.final.append(tok)
        return tok

    def load(self, q, out_t, out_ap, in_ap, r=(), lane=None):
        return self.dma(q, lambda e: e.dma_start(out=out_ap, in_=in_ap), r=r, w=[out_t],
                        lane=lane or out_t.name)

    def store(self, q, out_ap, in_t, in_ap, w=(), lane=None, final=False):
        return self.dma(q, lambda e: e.dma_start(out=out_ap, in_=in_ap), r=[in_t], w=w,
                        lane=lane or in_t.name, final=final)

    def finish(self):
        waits = []
        seen = self.seen["sp"]
        for (sem, key, val) in self.final:
            if seen.get(key, -1) >= val:
                continue
            seen[key] = val
            waits.append((sem, val))

        def emit(e, waits=waits):
            for (s, v) in waits:
                e.wait_ge(s, v)

        self.q["sp"].append(emit)
        q = self.q
        with self.nc.Block() as blk:
            @blk.sync
            def _(e):
                for f in q["sp"]:
                    f(e)

            @blk.tensor
            def _(e):
                for f in q["pe"]:
                    f(e)

            @blk.scalar
            def _(e):
                for f in q["act"]:
                    f(e)

            @blk.vector
            def _(e):
                for f in q["dve"]:
                    f(e)

            @blk.gpsimd
            def _(e):
                for f in q["pool"]:
                    f(e)


def emit_rstd(P, x_t, scr_t, ss_t, rs_t, epsb, n, eng_sq="act"):
    x_ap, x_buf = x_t
    P.op("act", lambda e: e.activation(out=scr_t[:, 0:n], in_=x_ap, func=AF.Square, accum_out=ss_t[:, 0:1]),
         r=[x_buf], w=[scr_t, ss_t])
    P.op("act", lambda e: e.activation(out=ss_t[:, 0:1], in_=ss_t[:, 0:1], func=AF.Sqrt, scale=1.0 / n,
                                       bias=epsb[:, 0:1]), r=[ss_t, epsb], w=[ss_t])
    P.op("dve", lambda e: e.reciprocal(out=rs_t[:, 0:1], in_=ss_t[:, 0:1]), r=[ss_t], w=[rs_t])


def emit_transposes(P, src_t, dst_t, ps_t, ident_ap, ident_buf, n, evac="dve", dst_off=0, width=128):
    psb = ps_t[:, :].bitcast(BF16)

    def pe(e):
        ins = None
        for k in range(n):
            ins = e.transpose(out=psb[:, k * 128:(k + 1) * 128], in_=src_t[:, k * 128:(k + 1) * 128],
                              identity=ident_ap)
        return ins

    P.op("pe", pe, r=[src_t, ident_buf], w=[ps_t])
    src_v = psb[:, 0:n * 128].rearrange("p (k t) -> p k t", k=n)
    if evac == "dve":
        P.op("dve", lambda e: e.tensor_copy(out=dst_t[:, 0:n, dst_off:dst_off + 128], in_=src_v), r=[ps_t], w=[dst_t])
    else:
        P.op("act", lambda e: e.copy(out=dst_t[:, 0:n, dst_off:dst_off + 128], in_=src_v), r=[ps_t], w=[dst_t])


def moe_layer(P, pfx, xs, xs_deps, PS, C, W, post):
    nc = P.nc
    n = pfx
    xt = [P.shared(f"xt{s}", [128, D], F32) for s in range(2)]
    hb = [P.shared(f"hb{s}", [128, D], BF16) for s in range(2)]
    scr = P.shared("scr", [128, D], F32)
    ss = P.sb(f"{n}ss", [128, 1], F32)
    rstd = P.sb(f"{n}rstd", [128, 1], F32)
    gbc = P.sb(f"{n}gbc", [128, D], F32)
    gfm = P.sb(f"{n}gfm", [128, 8], F32)
    wr = P.sb(f"{n}wr", [128, 8, 36], F32)
    brbc = P.sb(f"{n}brbc", [128, 36], F32)
    xT = P.sb(f"{n}xT", [128, 8, 128], F32)
    L = P.sb(f"{n}L", [128, 36], F32)
    sm = P.sb(f"{n}sm", [128, 16], F32)
    ohg = P.sb(f"{n}ohg", [128, 4], F32)
    eg = P.sb(f"{n}eg", [128, 4], F32)
    pen = P.sb(f"{n}pen", [128, 4], F32)
    Lm = P.sb(f"{n}Lm", [128, 32], F32)
    Lm2 = P.sb(f"{n}Lm2", [128, 32], F32)
    oh = [P.sb(f"{n}oh{k}", [128, 32], F32) for k in range(2)]
    ind = P.sb(f"{n}ind", [128, 32], BF16)
    cnt = P.sb(f"{n}cnt", [128, 32], F32)
    posv = P.sb(f"{n}posv", [128, 32], F32)
    ovf = P.sb(f"{n}ovf", [128, 32], F32)
    tmp32 = P.sb(f"{n}tmp32", [128, 32], F32)
    slotf = P.sb(f"{n}slotf", [128, 2], F32)
    gclf = P.sb(f"{n}gclf", [128, 2], F32)
    val = P.sb(f"{n}val", [128, 2], F32)
    slot_i = [[P.sb(f"{n}sl{i}_{k}", [128, 1], I32) for k in range(2)] for i in range(NT)]
    gidx_i = [[P.sb(f"{n}gi{i}_{k}", [128, 1], I32) for k in range(2)] for i in range(NT)]
    gates = [P.sb(f"{n}gt{i}", [128, 2], F32) for i in range(NT)]
    NWS = 3
    wg = [P.sb(f"{n}wg{s}", [128, 8, 512], BF16) for s in range(NWS)]
    wu = [P.sb(f"{n}wu{s}", [128, 8, 512], BF16) for s in range(NWS)]
    wd = [P.sb(f"{n}wd{s}", [128, 4, 1024], BF16) for s in range(NWS)]
    xe = P.sb(f"{n}xe", [128, 2, D], BF16)
    xeT = P.sb(f"{n}xeT", [128, 8, 256], BF16)
    HT = P.sb(f"{n}HT", [128, 4, 256], BF16)
    sg = P.shared("sg", [128, D], F32)
    yo = [P.sb(f"{n}yo{j}", [128, D], F32) for j in range(2)]
    y01 = [P.sb(f"{n}yg{k}", [128, D], F32) for k in range(2)]
    xo = [P.shared(f"xo{s}", [128, D], F32) for s in range(2)]
    Xd = P.dram(f"{n}Xd", [NSLOT, D], BF16)
    Yd = P.dram(f"{n}Yd", [NSLOT, D], F32)
    Xd_parts = [Tn(None, f"{n}Xdp{j}") for j in range(2 * NT)]
    zt = P.sb(f"{n}zt", [128, D], BF16)
    Xd_zero = Tn(None, f"{n}Xd_zero")
    P.op("pool", lambda e: e.memset(zt[:], 0.0), w=[zt])
    nzb = NSLOT // 128
    for zb in range(nzb):
        P.dma("sp", lambda e, zb=zb: e.dma_start(
            out=Xd[zb * 128:(zb + 1) * 128, :], in_=zt[:]),
            r=[zt], w=[Xd_zero] if zb == nzb - 1 else [], lane=f"{n}zfill")
    Yd_parts = [Tn(None, f"{n}Ydp{e}") for e in range(2 * E)]

    ident_b, U_b, ones_b, ident_f, ebase, epsb = C["ident_b"], C["U_b"], C["ones_b"], C["ident_f"], C["ebase"], C["epsb"]
    cb, cf = C["cb"], C["cf"]

    def load_expert_gu(e_):
        s = e_ % NWS
        P.dma("pool", lambda e: e.dma_start(out=wg[s][:], in_=W["w_gate"][e_].rearrange("(k p) f -> p k f", p=128)),
              w=[wg[s]], lane=wg[s].name)
        P.dma("pool", lambda e: e.dma_start(out=wu[s][:], in_=W["w_up"][e_].rearrange("(k p) f -> p k f", p=128)),
              w=[wu[s]], lane=wu[s].name)

    def load_expert_d(e_):
        s = e_ % NWS
        P.dma("pool", lambda e: e.dma_start(out=wd[s][:], in_=W["w_down"][e_].rearrange("(k p) f -> p k f", p=128)),
              w=[wd[s]], lane=wd[s].name)

    def load_expert(e_):
        load_expert_gu(e_)
        load_expert_d(e_)

    for e0 in range(NWS):
        load_expert(e0)

    P.load("sp", gbc, gbc[:], W["g_bc"][:, :])
    P.load("sp", gfm, gfm[:], W["g_fm"][:, :])
    P.load("sp", wr, wr[:], W["wr"].rearrange("(k p) f -> p k f", p=128))
    P.load("sp", brbc, brbc[:], W["br_bc"][:, :])
    for k in range(8):
        P.op("dve", lambda e, k=k: e.tensor_scalar(out=wr[:, k, :], in0=wr[:, k, :], scalar1=gfm[:, k:k + 1],
                                                   scalar2=None, op0=ALU.mult), r=[wr, gfm], w=[wr])
    P.op("dve", lambda e: e.memset(cnt[:], 0.0), w=[cnt])

    pTa, pTb, pL, pPos, pCnt = PS[0], PS[1], PS[2], PS[3], PS[4]

    for i in range(NT):
        s = i % 2
        x_ = xt[s]
        h_ = hb[s]
        P.load("sp", x_, x_[:], xs[i * 128:(i + 1) * 128, :], r=xs_deps)
        emit_rstd(P, (x_[:], x_), scr, ss, rstd, epsb, D)
        P.op("dve", lambda e, x_=x_, h_=h_: e.scalar_tensor_tensor(out=h_[:], in0=x_[:], scalar=rstd[:, 0:1], in1=gbc[:],
                                                                     op0=ALU.mult, op1=ALU.mult),
             r=[x_, rstd, gbc], w=[h_])
        for half, pt in ((0, pTa), (1, pTb)):
            def pe(e, half=half, pt=pt, x_=x_):
                ins = None
                for kk in range(4):
                    k = half * 4 + kk
                    ins = e.transpose(out=pt[:, kk * 128:(kk + 1) * 128], in_=x_[:, k * 128:(k + 1) * 128],
                                      identity=ident_f)
                return ins
            P.op("pe", pe, r=[x_, cf], w=[pt])
            P.op("act", lambda e, half=half, pt=pt: e.copy(out=xT[:, half * 4:(half + 1) * 4, :],
                                                           in_=pt[:, :].rearrange("p (k t) -> p k t", k=4)),
                 r=[pt], w=[xT])

        def pe_logits(e):
            ins = None
            for k in range(8):
                ins = e.matmul(pL[:, 0:36], lhsT=xT[:, k, :], rhs=wr[:, k, :], start=(k == 0), stop=(k == 7))
            return ins
        P.op("pe", pe_logits, r=[xT, wr], w=[pL])
        P.op("dve", lambda e: e.scalar_tensor_tensor(out=L[:], in0=pL[:, 0:36], scalar=rstd[:, 0:1], in1=brbc[:],
                                                     op0=ALU.mult, op1=ALU.add), r=[pL, rstd, brbc], w=[L])
        P.op("dve", lambda e: e.reduce_max(out=sm[:, 0:1], in_=L[:, 0:4], axis=AX.X), r=[L], w=[sm])
        P.op("dve", lambda e: e.tensor_scalar(out=ohg[:], in0=L[:, 0:4], scalar1=sm[:, 0:1], scalar2=None,
                                              op0=ALU.is_equal), r=[L, sm], w=[ohg])
        P.op("dve", lambda e: e.tensor_scalar(out=sm[:, 1:2], in0=sm[:, 0:1], scalar1=-1.0, scalar2=None,
                                              op0=ALU.mult), r=[sm], w=[sm])
        P.op("act", lambda e: e.activation(out=eg[:], in_=L[:, 0:4], func=AF.Exp, bias=sm[:, 1:2], scale=1.0,
                                           accum_out=sm[:, 2:3]), r=[L, sm], w=[eg, sm])
        P.op("dve", lambda e: e.reciprocal(out=sm[:, 3:4], in_=sm[:, 2:3]), r=[sm], w=[sm])
        P.op("dve", lambda e: e.tensor_scalar(out=pen[:], in0=ohg[:], scalar1=BIG, scalar2=-BIG, op0=ALU.mult,
                                              op1=ALU.add), r=[ohg], w=[pen])

        def masklog(e):
            ins = None
            for g in range(4):
                ins = e.tensor_scalar(out=Lm[:, g * 8:(g + 1) * 8], in0=L[:, 4 + g * 8:12 + g * 8],
                                      scalar1=pen[:, g:g + 1], scalar2=None, op0=ALU.add)
            return ins
        P.op("dve", masklog, r=[L, pen], w=[Lm])
        P.op("dve", lambda e: e.reduce_max(out=sm[:, 4:5], in_=Lm[:], axis=AX.X), r=[Lm], w=[sm])
        P.op("dve", lambda e: e.tensor_scalar(out=oh[0][:], in0=Lm[:], scalar1=sm[:, 4:5], scalar2=None,
                                              op0=ALU.is_equal), r=[Lm, sm], w=[oh[0]])
        P.op("dve", lambda e: e.scalar_tensor_tensor(out=Lm2[:], in0=oh[0][:], scalar=-BIG, in1=Lm[:],
                                                     op0=ALU.mult, op1=ALU.add), r=[oh[0], Lm], w=[Lm2])
        P.op("dve", lambda e: e.reduce_max(out=sm[:, 5:6], in_=Lm2[:], axis=AX.X), r=[Lm2], w=[sm])
        P.op("dve", lambda e: e.tensor_scalar(out=oh[1][:], in0=Lm2[:], scalar1=sm[:, 5:6], scalar2=None,
                                              op0=ALU.is_equal), r=[Lm2, sm], w=[oh[1]])
        P.op("dve", lambda e: e.tensor_tensor(out=sm[:, 6:7], in0=sm[:, 5:6], in1=sm[:, 4:5], op=ALU.subtract),
             r=[sm], w=[sm])
        P.op("act", lambda e: e.activation(out=sm[:, 7:8], in_=sm[:, 6:7], func=AF.Exp), r=[sm], w=[sm])
        P.op("dve", lambda e: e.tensor_scalar(out=sm[:, 8:9], in0=sm[:, 7:8], scalar1=1.0, scalar2=None,
                                              op0=ALU.add), r=[sm], w=[sm])
        P.op("dve", lambda e: e.reciprocal(out=sm[:, 9:10], in_=sm[:, 8:9]), r=[sm], w=[sm])
        gt = gates[i]
        P.op("dve", lambda e, gt=gt: e.tensor_tensor(out=gt[:, 0:1], in0=sm[:, 3:4], in1=sm[:, 9:10], op=ALU.mult),
             r=[sm], w=[gt])
        P.op("dve", lambda e, gt=gt: e.tensor_tensor(out=gt[:, 1:2], in0=gt[:, 0:1], in1=sm[:, 7:8], op=ALU.mult),
             r=[sm, gt], w=[gt])
        P.op("dve", lambda e: e.tensor_tensor(out=ind[:], in0=oh[0][:], in1=oh[1][:], op=ALU.add),
             r=[oh[0], oh[1]], w=[ind])
        P.op("pe", lambda e: e.matmul(pPos[:, 0:32], lhsT=U_b, rhs=ind[:], start=True, stop=True),
             r=[ind, cb], w=[pPos])
        P.op("pe", lambda e: e.matmul(pCnt[:, 0:32], lhsT=ones_b, rhs=ind[:], start=True, stop=True),
             r=[ind, cb], w=[pCnt])
        P.op("dve", lambda e: e.tensor_tensor(out=posv[:], in0=pPos[:, 0:32], in1=cnt[:], op=ALU.add),
             r=[pPos, cnt], w=[posv])
        P.op("dve", lambda e: e.tensor_tensor(out=cnt[:], in0=pCnt[:, 0:32], in1=cnt[:], op=ALU.add),
             r=[pCnt, cnt], w=[cnt])
        P.op("dve", lambda e: e.tensor_scalar(out=ovf[:], in0=posv[:], scalar1=float(CAP), scalar2=1.0e6,
                                              op0=ALU.is_ge, op1=ALU.mult), r=[posv], w=[ovf])
        P.op("dve", lambda e: e.tensor_tensor(out=posv[:], in0=posv[:], in1=ebase, op=ALU.add), r=[posv, cf], w=[posv])
        P.op("dve", lambda e: e.tensor_tensor(out=posv[:], in0=posv[:], in1=ovf[:], op=ALU.add), r=[posv, ovf], w=[posv])
        for k in range(2):
            P.op("dve", lambda e, k=k: e.tensor_tensor(out=tmp32[:], in0=oh[k][:], in1=posv[:], op=ALU.mult),
                 r=[oh[k], posv], w=[tmp32])
            P.op("dve", lambda e, k=k: e.reduce_sum(out=slotf[:, k:k + 1], in_=tmp32[:], axis=AX.X),
                 r=[tmp32], w=[slotf])
        P.op("dve", lambda e: e.tensor_scalar(out=val[:], in0=slotf[:], scalar1=1.0e5, scalar2=None, op0=ALU.is_lt),
             r=[slotf], w=[val])
        P.op("dve", lambda e, gt=gt: e.tensor_tensor(out=gt[:], in0=gt[:], in1=val[:], op=ALU.mult), r=[gt, val], w=[gt])
        P.op("dve", lambda e: e.tensor_scalar(out=gclf[:], in0=slotf[:], scalar1=float(NSLOT - 1), scalar2=None,
                                              op0=ALU.min), r=[slotf], w=[gclf])
        for k in range(2):
            si = slot_i[i][k]
            gi = gidx_i[i][k]
            P.op("dve", lambda e, k=k, si=si: e.tensor_copy(out=si[:], in_=slotf[:, k:k + 1]), r=[slotf], w=[si])
            P.op("dve", lambda e, k=k, gi=gi: e.tensor_copy(out=gi[:], in_=gclf[:, k:k + 1]), r=[gclf], w=[gi])
            xp = Xd_parts[2 * i + k]
            P.dma("pool", lambda e, si=si, h_=h_: e.indirect_dma_start(
                out=Xd[:, :], out_offset=bass.IndirectOffsetOnAxis(ap=si[:, :], axis=0),
                in_=h_[:, :], in_offset=None, bounds_check=P.bc_reg(e), oob_is_err=False),
                r=[h_, si, Xd_zero], w=[xp], lane=f"{n}sc{s}_{k}")

    pT, pG0, pG1, pU0, pU1, pY0, pY1 = PS[0], PS[1], PS[2], PS[3], PS[4], PS[5], PS[6]
    pTj = (PS[0], PS[7])

    def emit_T_pe(e_):
        P.dma("sp", lambda e, e_=e_: e.dma_start(out=xe[:], in_=Xd[e_ * CAP:(e_ + 1) * CAP, :].rearrange(
            "(j p) c -> p j c", p=128)), r=Xd_parts, w=[xe], lane=xe.name)
        for j in range(2):
            pt_ = pTj[j]
            psb = pt_[:, :].bitcast(BF16)

            def pe(e, j=j, psb=psb):
                ins = None
                for k in range(8):
                    ins = e.transpose(out=psb[:, k * 128:(k + 1) * 128], in_=xe[:, j, k * 128:(k + 1) * 128],
                                      identity=ident_b)
                return ins
            P.op("pe", pe, r=[xe, cb], w=[pt_])

    def emit_T_copy(e_):
        for j in range(2):
            psb = pTj[j][:, :].bitcast(BF16)
            P.op("dve", lambda e, j=j, psb=psb: e.tensor_copy(out=xeT[:, :, j * 128:(j + 1) * 128],
                                                             in_=psb.rearrange("p (k t) -> p k t", k=8)),
                 r=[pTj[j]], w=[xeT])

    emit_T_pe(0)
    emit_T_copy(0)
    for e_ in range(E):
        s = e_ % NWS
        for (wt, p0, p1) in ((wg[s], pG0, pG1), (wu[s], pU0, pU1)):
            for f in range(4):
                pt = p0 if f < 2 else p1

                def pe(e, wt=wt, f=f, pt=pt):
                    ins = None
                    for k in range(8):
                        ins = e.matmul(pt[:, (f % 2) * 256:(f % 2 + 1) * 256], lhsT=wt[:, k, f * 128:(f + 1) * 128],
                                       rhs=xeT[:, k, :], start=(k == 0), stop=(k == 7))
                    return ins
                P.op("pe", pe, r=[wt, xeT], w=[pt])
        if e_ + NWS < E:
            load_expert_gu(e_ + NWS)
        if e_ + 1 < E:
            emit_T_pe(e_ + 1)
        for half, (pg, pu) in enumerate(((pG0, pU0), (pG1, pU1))):
            P.op("act", lambda e, half=half, pg=pg: e.activation(
                out=sg[:, half * 512:(half + 1) * 512], in_=pg[:, :], func=AF.Silu),
                r=[pg], w=[sg])
            P.op("dve", lambda e, half=half, pu=pu: e.tensor_tensor(
                out=HT[:, half * 2:(half + 1) * 2, :], in0=sg[:, half * 512:(half + 1) * 512].rearrange("p (f r) -> p f r", f=2),
                in1=pu[:, :].rearrange("p (f r) -> p f r", f=2), op=ALU.mult), r=[sg, pu], w=[HT])
        if e_ + 1 < E:
            emit_T_copy(e_ + 1)
        for j in range(2):
            for dh, py in ((0, pY0), (1, pY1)):
                def pe(e, j=j, dh=dh, py=py, s=s):
                    ins = None
                    for f in range(4):
                        ins = e.matmul(py[:, :], lhsT=HT[:, f, j * 128:(j + 1) * 128],
                                       rhs=wd[s][:, f, dh * 512:(dh + 1) * 512], start=(f == 0), stop=(f == 3))
                    return ins
                P.op("pe", pe, r=[HT, wd[s]], w=[py])
                if dh == 0:
                    P.op("act", lambda e, j=j, py=py: e.copy(out=yo[j][:, 0:512], in_=py[:, :]), r=[py], w=[yo[j]])
                else:
                    P.op("dve", lambda e, j=j, py=py: e.tensor_copy(out=yo[j][:, 512:1024], in_=py[:, :]),
                         r=[py, yo[j]], w=[yo[j]])
            P.dma("sp", lambda e, j=j, e_=e_: e.dma_start(out=Yd[e_ * CAP + j * 128:e_ * CAP + (j + 1) * 128, :],
                                                         in_=yo[j][:]), r=[yo[j]], w=[Yd_parts[2 * e_ + j]],
                  lane=yo[j].name)
        if e_ + NWS < E:
            load_expert_d(e_ + NWS)
    P.load("sp", xt[0], xt[0][:], xs[0:128, :], r=xs_deps)
    for i in range(NT):
        s = i % 2
        x_ = xt[s]
        if i + 1 < NT:
            xn = xt[(i + 1) % 2]
            P.load("sp", xn, xn[:], xs[(i + 1) * 128:(i + 2) * 128, :], r=xs_deps)
        for k in range(2):
            gi = gidx_i[i][k]
            P.dma("pool", lambda e, k=k, gi=gi: e.indirect_dma_start(
                out=y01[k][:, :], out_offset=None, in_=Yd[:, :],
                in_offset=bass.IndirectOffsetOnAxis(ap=gi[:, :], axis=0), bounds_check=P.bc_reg(e), oob_is_err=False),
                r=Yd_parts + [gi], w=[y01[k]], lane=y01[k].name)
        gt = gates[i]
        xo_ = xo[s]
        P.op("dve", lambda e, x_=x_, gt=gt, xo_=xo_: e.scalar_tensor_tensor(
            out=xo_[:], in0=y01[0][:], scalar=gt[:, 0:1], in1=x_[:], op0=ALU.mult, op1=ALU.add),
            r=[y01[0], gt, x_], w=[xo_])
        P.op("dve", lambda e, gt=gt, xo_=xo_: e.scalar_tensor_tensor(
            out=xo_[:], in0=y01[1][:], scalar=gt[:, 1:2], in1=xo_[:], op0=ALU.mult, op1=ALU.add),
            r=[y01[1], gt, xo_], w=[xo_])
        post(i, xo_)


def load_consts(P, cb_d, cf_d):
    cb = P.sb("cb_s", [128, 3 * 128], BF16)
    cf = P.sb("cf_s", [128, 128 + 32], F32)
    epsb = P.sb("epsb", [128, 1], F32)
    P.load("sp", cb, cb[:], cb_d[:, :])
    P.load("sp", cf, cf[:], cf_d[:, :])
    P.op("dve", lambda e: e.memset(epsb[:], EPS), w=[epsb])
    return dict(cb=cb, cf=cf, epsb=epsb, ident_b=cb[:, 0:128], U_b=cb[:, 128:256], ones_b=cb[:, 256:384],
                ident_f=cf[:, 0:128], ebase=cf[:, 128:160])


def host_consts():
    cb = np.zeros((128, 384), np.float32)
    cb[:, 0:128] = np.eye(128)
    cb[:, 128:256] = np.triu(np.ones((128, 128)), 1)
    cb[:, 256:384] = 1.0
    cf = np.zeros((128, 160), np.float32)
    cf[:, 0:128] = np.eye(128)
    cf[:, 128:160] = (np.arange(32) * CAP)[None, :]
    return cb.astype(NPBF), cf


def moe_dram_inputs(nc, pfx):
    W = {}
    W["g_bc"] = nc.dram_tensor(pfx + "g_bc", [128, D], F32, kind="ExternalInput")
    W["g_fm"] = nc.dram_tensor(pfx + "g_fm", [128, 8], F32, kind="ExternalInput")
    W["wr"] = nc.dram_tensor(pfx + "wr", [D, 36], F32, kind="ExternalInput").ap()
    W["br_bc"] = nc.dram_tensor(pfx + "br_bc", [128, 36], F32, kind="ExternalInput")
    W["w_gate"] = nc.dram_tensor(pfx + "w_gate", [E, D, 512], F32, kind="ExternalInput")
    W["w_up"] = nc.dram_tensor(pfx + "w_up", [E, D, 512], F32, kind="ExternalInput")
    W["w_down"] = nc.dram_tensor(pfx + "w_down", [E, 512, D], F32, kind="ExternalInput")
    return W


def moe_host_inputs(pfx, l, inp):
    g = inp["ffn_norm"][l]
    return {
        pfx + "g_bc": np.ascontiguousarray(np.broadcast_to(g[None, :], (128, D))),
        pfx + "g_fm": np.ascontiguousarray(g.reshape(8, 128).T),
        pfx + "wr": np.ascontiguousarray(np.concatenate([inp["router_group_w"][l], inp["router_expert_w"][l]], axis=1)),
        pfx + "br_bc": np.ascontiguousarray(np.broadcast_to(
            np.concatenate([inp["router_group_b"][l], inp["router_expert_b"][l]])[None, :], (128, 36))),
        pfx + "w_gate": inp["w_gate"][l],
        pfx + "w_up": inp["w_up"][l],
        pfx + "w_down": inp["w_down"][l],
    }


def declare_A(nc):
    io = {}
    io["xin"] = nc.dram_tensor("xin", [TPC + 128, D], F32, kind="ExternalInput")
    io["cb"] = nc.dram_tensor("cb", [128, 384], BF16, kind="ExternalInput")
    io["cf"] = nc.dram_tensor("cf", [128, 160], F32, kind="ExternalInput")
    io["pvec"] = nc.dram_tensor("pvec", [3, 128, D], F32, kind="ExternalInput")
    io["pool_w"] = nc.dram_tensor("pool_w", [4, 256, 256], F32, kind="ExternalInput")
    io["amain"] = nc.dram_tensor("amain", [128, 8, 128], BF16, kind="ExternalInput")
    io["ahalo"] = nc.dram_tensor("ahalo", [128, 8, 128], BF16, kind="ExternalInput")
    io["invc"] = nc.dram_tensor("invc", [128, 8, 128], F32, kind="ExternalInput")
    io["lvec"] = nc.dram_tensor("lvec", [2, 128, D], F32, kind="ExternalInput")
    io["lvec2"] = nc.dram_tensor("lvec2", [128, 640], F32, kind="ExternalInput")
    io["w_dkv"] = nc.dram_tensor("w_dkv", [D, 320], F32, kind="ExternalInput")
    io["wq_down"] = nc.dram_tensor("wq_down", [D, 384], F32, kind="ExternalInput")
    io["cs"] = nc.dram_tensor("cs", [128, NT, 64], F32, kind="ExternalInput")
    io["W0"] = moe_dram_inputs(nc, "m0_")
    io["x2"] = nc.dram_tensor("x2", [TPC, D], F32)
    io["latT"] = [nc.dram_tensor(f"latT{k}", [128, TPC], BF16) for k in range(6)]
    io["xs1"] = nc.dram_tensor("xs1", [TPC, D], F32)
    return io


def phase_A_pool(P, nc, PS, C, io):
    xin, pvec_d, poolw_d = io["xin"], io["pvec"], io["pool_w"]
    amain_d, ahalo_d, invc_d = io["amain"], io["ahalo"], io["invc"]
    xs1 = io["xs1"]
    epsb = C["epsb"]

    NXT = 4
    xt = [P.sb(f"pxt{s}", [128, D], F32) for s in range(NXT)]
    hb = [P.shared(f"hb{s}", [128, D], BF16) for s in range(2)]
    scr = P.shared("scr", [128, D], F32)
    ss2 = [P.sb(f"pss{s}", [128, 1], F32) for s in range(2)]
    rstd2 = [P.sb(f"prstd{s}", [128, 1], F32) for s in range(2)]
    pvec = [P.shared(f"vec{j}", [128, D], F32) for j in range(3)]
    pw = P.sb("pw", [128, 4, 2, 256], BF16)
    amain = P.sb("amain_s", [128, 8, 128], BF16)
    ahalo = P.sb("ahalo_s", [128, 8, 128], BF16)
    invc = P.sb("invc_s", [128, 8, 128], F32)
    pooledT2 = [P.sb(f"pooledT{s}", [128, 8, 128], BF16) for s in range(2)]
    tmpf2 = [P.sb(f"ptmpf{s}", [128, D], F32) for s in range(2)]
    xo1 = [P.shared(f"xo{s}", [128, D], F32) for s in range(2)]
    for j in range(3):
        P.load("sp", pvec[j], pvec[j][:], pvec_d[j, :, :])
    P.dma("pool", lambda e: e.dma_start(out=pw[:], in_=poolw_d.ap().rearrange("g (kk p) d -> p g kk d", p=128)),
          w=[pw], lane="pw")
    P.load("sp", amain, amain[:], amain_d[:, :, :])
    P.load("sp", ahalo, ahalo[:], ahalo_d[:, :, :])
    P.load("sp", invc, invc[:], invc_d[:, :, :])
    xs1_parts = [Tn(None, f"xs1p{i}") for i in range(NT)]

    def tile_state(i):
        s = (i + 1) % 2
        return dict(s=s, x_=xt[(i + 1) % NXT], h_=hb[s], hp=hb[1 - s], ss=ss2[s], rstd=rstd2[s], pooledT=pooledT2[s],
                    tmpf=tmpf2[s], bank=(PS[0:4] if s == 0 else PS[4:8]))

    def head(i):
        st = tile_state(i)
        x_, h_, hp, ss, rstd, bank = st["x_"], st["h_"], st["hp"], st["ss"], st["rstd"], st["bank"]
        emit_rstd(P, (x_[:], x_), scr, ss, rstd, epsb, D)
        P.op("dve", lambda e: e.scalar_tensor_tensor(out=h_[:], in0=x_[:], scalar=rstd[:, 0:1], in1=pvec[0][:],
                                                     op0=ALU.mult, op1=ALU.mult), r=[x_, rstd, pvec[0]], w=[h_])
        if i < 0:
            return
        v = 0 if i == 0 else 1
        for half, pp in ((0, bank[0]), (1, bank[1])):
            def pe(e, half=half, pp=pp):
                ins = None
                for kk in range(4):
                    k = half * 4 + kk
                    blk = v * 4 + k // 2
                    e.matmul(pp[:, kk * 128:(kk + 1) * 128], lhsT=h_[:, k * 128:(k + 1) * 128],
                             rhs=amain[:, blk, :], start=True, stop=False)
                    ins = e.matmul(pp[:, kk * 128:(kk + 1) * 128], lhsT=hp[64:128, k * 128:(k + 1) * 128],
                                   rhs=ahalo[64:128, blk, :], start=False, stop=True)
                return ins
            P.op("pe", pe, r=[h_, hp, amain, ahalo], w=[pp])

    def tail(i):
        st = tile_state(i)
        x_, pooledT, tmpf, bank = st["x_"], st["pooledT"], st["tmpf"], st["bank"]
        v = 0 if i == 0 else 1
        for half, pp in ((0, bank[0]), (1, bank[1])):
            def ev(e, half=half, pp=pp):
                ins = None
                for kk in range(4):
                    k = half * 4 + kk
                    blk = v * 4 + k // 2
                    ins = e.tensor_tensor(out=pooledT[:, k, :], in0=pp[:, kk * 128:(kk + 1) * 128],
                                          in1=invc[:, blk, :], op=ALU.mult)
                return ins
            P.op("dve", ev, r=[pp, invc], w=[pooledT])
        for half, py in ((0, bank[2]), (1, bank[3])):
            def pe(e, half=half, py=py):
                ins = None
                for gg in range(2):
                    g = half * 2 + gg
                    for kk in range(2):
                        ins = e.matmul(py[:, gg * 256:(gg + 1) * 256], lhsT=pooledT[:, 2 * g + kk, :],
                                       rhs=pw[:, g, kk, :], start=(kk == 0), stop=(kk == 1))
                return ins
            P.op("pe", pe, r=[pooledT, pw], w=[py])
            P.op("dve", lambda e, half=half, py=py: e.tensor_tensor(
                out=tmpf[:, half * 512:(half + 1) * 512], in0=py[:, :], in1=pvec[1][:, half * 512:(half + 1) * 512],
                op=ALU.add), r=[py, pvec[1], tmpf], w=[tmpf])
        xo_ = xo1[i % 2]
        P.op("pool", lambda e: e.tensor_tensor(out=tmpf[:], in0=tmpf[:], in1=pvec[2][:], op=ALU.mult),
             r=[tmpf, pvec[2]], w=[tmpf])
        P.op("pool", lambda e: e.tensor_tensor(out=xo_[:], in0=tmpf[:], in1=x_[:], op=ALU.add), r=[tmpf, x_], w=[xo_])
        P.store("sp", xs1[i * 128:(i + 1) * 128, :], xo_, xo_[:], w=[xs1_parts[i]])

    def load(i):
        x_ = xt[(i + 1) % NXT]
        P.load("sp", x_, x_[:], xin[(i + 1) * 128:(i + 2) * 128, :])

    AHEAD = NXT - 2
    for i in range(-1, min(NT, -1 + AHEAD + 1)):
        load(i)
    head(-1)
    head(0)
    for i in range(NT):
        if i + AHEAD < NT:
            load(i + AHEAD)
        if i + 1 < NT:
            head(i + 1)
        tail(i)
    io["xs1_parts"] = xs1_parts


def phase_A_moe(P, nc, PS, C, io):
    lvec_d, lvec2_d = io["lvec"], io["lvec2"]
    wdkv_d, wqd_d, cs_d, W, x2_d, xs1 = io["w_dkv"], io["wq_down"], io["cs"], io["W0"], io["x2"], io["xs1"]
    latT_d = io["latT"]
    xs1_parts = io["xs1_parts"]
    epsb = C["epsb"]
    lv = [P.shared(f"vec{j}", [128, D], F32) for j in range(2)]
    lv2 = P.sb("lv2", [128, 640], F32)
    wdkv = P.sb("wdkv", [128, 8, 320], BF16)
    wqd = P.sb("wqd", [128, 8, 384], BF16)
    cs = P.sb("cs_s", [128, NT, 64], F32)
    hk = P.sb("hk", [128, D], BF16)
    hq = P.sb("hq", [128, D], BF16)
    hkT = P.sb("hkT", [128, 8, 128], BF16)
    hqT = P.sb("hqT", [128, 8, 128], BF16)
    lat = P.sb("lat", [128, 768], BF16)
    latT = P.sb("latT_s", [128, 6, TPC], BF16)
    ss2 = P.sb("lss2", [128, 1], F32)
    rs2 = P.sb("lrs2", [128, 1], F32)
    ss3 = P.sb("lss3", [128, 1], F32)
    rs3 = P.sb("lrs3", [128, 1], F32)
    ssl = P.sb("lssl", [128, 1], F32)
    rsl = P.sb("lrsl", [128, 1], F32)
    lscr = P.shared("scr", [128, D], F32)
    rt = P.sb("rt", [128, 4, 32], F32)
    for j in range(2):
        P.load("sp", lv[j], lv[j][:], lvec_d[j, :, :])
    P.load("sp", lv2, lv2[:], lvec2_d[:, :])
    P.load("sp", cs, cs[:], cs_d[:, :, :])
    P.dma("pool", lambda e: e.dma_start(out=wdkv[:], in_=wdkv_d.ap().rearrange("(k p) f -> p k f", p=128)),
          w=[wdkv], lane="wdkv")
    P.dma("pool", lambda e: e.dma_start(out=wqd[:], in_=wqd_d.ap().rearrange("(k p) f -> p k f", p=128)),
          w=[wqd], lane="wqd")
    P.op("pool", lambda e: e.memset(lat[:, 704:768], 0.0), w=[lat])

    x2_parts = [Tn(None, f"x2p{i}") for i in range(NT)]
    io["x2_parts"] = x2_parts

    def post(i, xo_):
        P.store("sp", x2_d[i * 128:(i + 1) * 128, :], xo_, xo_[:], w=[x2_parts[i]])
        emit_rstd(P, (xo_[:], xo_), lscr, ssl, rsl, epsb, D)
        P.op("dve", lambda e: e.scalar_tensor_tensor(out=hk[:], in0=xo_[:], scalar=rsl[:, 0:1], in1=lv[0][:],
                                                     op0=ALU.mult, op1=ALU.mult), r=[xo_, rsl, lv[0]], w=[hk])
        P.op("dve", lambda e: e.scalar_tensor_tensor(out=hq[:], in0=xo_[:], scalar=rsl[:, 0:1], in1=lv[1][:],
                                                      op0=ALU.mult, op1=ALU.mult), r=[xo_, rsl, lv[1]], w=[hq])
        emit_transposes(P, hk, hkT, PS[5], C["ident_b"], C["cb"], 8, evac="act")
        emit_transposes(P, hq, hqT, PS[6], C["ident_b"], C["cb"], 8, evac="dve")
        pkv, pq = PS[7], PS[5]

        def pe1(e):
            ins = None
            for k in range(8):
                ins = e.matmul(pkv[:, 0:320], lhsT=hkT[:, k, :], rhs=wdkv[:, k, :], start=(k == 0), stop=(k == 7))
            return ins
        P.op("pe", pe1, r=[hkT, wdkv], w=[pkv])

        def pe2(e):
            ins = None
            for k in range(8):
                ins = e.matmul(pq[:, 0:384], lhsT=hqT[:, k, :], rhs=wqd[:, k, :], start=(k == 0), stop=(k == 7))
            return ins
        P.op("pe", pe2, r=[hqT, wqd], w=[pq])
        emit_rstd(P, (pkv[:, 0:256], pkv), lscr, ss2, rs2, epsb, 256)
        P.op("dve", lambda e: e.scalar_tensor_tensor(out=lat[:, 384:640], in0=pkv[:, 0:256], scalar=rs2[:, 0:1],
                                                     in1=lv2[:, 0:256], op0=ALU.mult, op1=ALU.mult),
             r=[pkv, rs2, lv2, lat], w=[lat])
        cosv = cs[:, i, 0:32]
        sinv = cs[:, i, 32:64]

        def rope(e):
            e.tensor_tensor(out=rt[:, 0, :], in0=pkv[:, 256:288], in1=cosv, op=ALU.mult)
            e.tensor_tensor(out=rt[:, 1, :], in0=pkv[:, 288:320], in1=sinv, op=ALU.mult)
            e.tensor_tensor(out=rt[:, 2, :], in0=pkv[:, 288:320], in1=cosv, op=ALU.mult)
            return e.tensor_tensor(out=rt[:, 3, :], in0=pkv[:, 256:288], in1=sinv, op=ALU.mult)
        P.op("dve", rope, r=[pkv, cs], w=[rt])

        def rope2(e):
            e.tensor_tensor(out=lat[:, 640:672], in0=rt[:, 0, :], in1=rt[:, 1, :], op=ALU.subtract)
            return e.tensor_tensor(out=lat[:, 672:704], in0=rt[:, 2, :], in1=rt[:, 3, :], op=ALU.add)
        P.op("dve", rope2, r=[rt, lat], w=[lat])
        emit_rstd(P, (pq[:, 0:384], pq), lscr, ss3, rs3, epsb, 384)
        P.op("dve", lambda e: e.scalar_tensor_tensor(out=lat[:, 0:384], in0=pq[:, 0:384], scalar=rs3[:, 0:1],
                                                     in1=lv2[:, 256:640], op0=ALU.mult, op1=ALU.mult),
             r=[pq, rs3, lv2, lat], w=[lat])
        emit_transposes(P, lat, latT, PS[6], C["ident_b"], C["cb"], 6, evac="act", dst_off=i * 128)

    moe_layer(P, "m0_", xs1, xs1_parts, PS, C, W, post)
    io["latT_done"] = [Tn(None, f"latT_done{k}") for k in range(6)]
    for k in range(6):
        P.dma("sp", lambda e, k=k: e.dma_start(out=latT_d[k][:, :], in_=latT[:, k, :]), r=[latT],
              w=[io["latT_done"][k]], lane=f"latT_out{k}")


def pool_consts(core):
    seq_start = (core % 4 == 0)
    wins = (2, 4, 8, 16)
    amain = np.zeros((128, 8, 128), np.float32)
    ahalo = np.zeros((128, 8, 128), np.float32)
    invc = np.zeros((128, 8, 128), np.float32)
    for v in range(2):
        for g, w in enumerate(wins):
            t = np.arange(128)
            if v == 0 and seq_start:
                cntv = np.minimum(t + 1, w).astype(np.float32)
            else:
                cntv = np.full(128, w, np.float32)
            B = np.zeros((128 + 64, 128), np.float32)
            for tt in range(128):
                lo = tt - w + 1
                B[lo + 64:tt + 64 + 1, tt] = 1.0
                B[tt + 64, tt] -= cntv[tt]
            amain[:, v * 4 + g, :] = B[64:, :]
            ahalo[64:, v * 4 + g, :] = B[:64, :]
            invc[:, v * 4 + g, :] = (1.0 / cntv)[None, :]
    return amain.astype(NPBF), ahalo.astype(NPBF), invc


def rope_np():
    half = 32
    inv = (np.float32(10000.0) ** (-np.arange(half, dtype=np.float32) / np.float32(half))).astype(np.float32)
    ang = np.arange(SEQ, dtype=np.float32)[:, None] * inv[None, :]
    return np.cos(ang).astype(np.float32), np.sin(ang).astype(np.float32)


_CACHE = {}

NQT = SEQ // 512


def declare_B(nc):
    io = {}
    io["wqn"] = nc.dram_tensor("wqn", [2, 384, 128], F32, kind="ExternalInput")
    io["wqr"] = nc.dram_tensor("wqr", [2, 384, 64], F32, kind="ExternalInput")
    io["wuk"] = nc.dram_tensor("wuk", [2, 256, 128], F32, kind="ExternalInput")
    io["wuv"] = nc.dram_tensor("wuv", [2, 256, 128], F32, kind="ExternalInput")
    io["csT"] = nc.dram_tensor("csT", [64, 2, SEQ], F32, kind="ExternalInput")
    io["mask"] = nc.dram_tensor("mask", [128, 4, 512], BF16, kind="ExternalInput")
    io["G"] = [nc.dram_tensor(f"latG{k}", [4 * 128, TPC], BF16) for k in range(6)]
    io["OTx"] = [nc.dram_tensor(f"OTx{q}", [128, TPC], BF16) for q in range(8)]
    io["OTg"] = nc.dram_tensor("OTall", [8 * 4 * 128, TPC], BF16)
    io["otidx"] = nc.dram_tensor("otidx", [128, 8], I32, kind="ExternalInput")
    return io


def phase_B(P, nc, PS, C, io):
    wqn_d, wqr_d, wuk_d, wuv_d, cs_d, mask_d = io["wqn"], io["wqr"], io["wuk"], io["wuv"], io["csT"], io["mask"]
    Gv = [g_.ap().rearrange("(r p) t -> r p t", r=4) for g_ in io["G"]]
    OTx = io["OTx"]
    Gdone = io["G_done"]
    ckvT = P.sb("ckvT", [128, 2, SEQ], BF16)
    krT = P.sb("krT", [128, SEQ], BF16)
    KT = P.sb("KT", [128, SEQ], BF16)
    V = P.sb("V", [128, 64, 128], BF16)
    cqt = [P.sb(f"cqt{s}", [128, 3, 512], BF16) for s in range(2)]
    cst = [P.sb(f"cst{s}", [128, 2, 512], F32) for s in range(2)]
    qn = [P.sb(f"qn{s}", [128, 512], BF16) for s in range(2)]
    qr = [P.sb(f"qr{s}", [128, 512], BF16) for s in range(2)]
    PT = [P.sb(f"PT{s}", [128, 512], BF16) for s in range(3)]
    mask = P.sb("mask_s", [128, 4, 512], BF16)
    t1 = P.sb("t1", [128, 512], F32)
    t2 = P.sb("t2", [128, 512], F32)
    rden = P.sb("rden", [128, 512], F32)
    oo = [P.sb(f"oo{s}", [128, 512], BF16) for s in range(2)]
    wqn = P.sb("wqn_s", [128, 2, 3, 128], BF16)
    wqr = P.sb("wqr_s", [128, 2, 3, 64], BF16)
    wqt = P.sb("wqt_s", [128, 2, 3, 64], BF16)
    wuk = P.sb("wuk_s", [128, 2, 2, 128], BF16)
    wuv = P.sb("wuv_s", [128, 2, 2, 128], BF16)

    for r_ in range(4):
        for c_ in range(2):
            P.load("sp", ckvT, ckvT[:, c_, r_ * TPC:(r_ + 1) * TPC], Gv[3 + c_][r_, :, :], r=[Gdone[3 + c_]])
        P.load("sp", krT, krT[0:64, r_ * TPC:(r_ + 1) * TPC], Gv[5][r_, 0:64, :], r=[Gdone[5]])
    P.load("sp", mask, mask[:], mask_d[:, :, :])
    ones_ap, ones_buf = C["ones_b"], C["cb"]
    for (t, d) in ((wqn, wqn_d), (wqr, wqr_d), (wuk, wuk_d), (wuv, wuv_d)):
        P.dma("pool", lambda e, t=t, d=d: e.dma_start(out=t[:], in_=d.ap().rearrange("h (k p) f -> p h k f", p=128)),
              w=[t], lane=t.name)
    P.op("dve", lambda e: e.tensor_scalar(out=wqt[:, :, :, 0:32], in0=wqr[:, :, :, 32:64], scalar1=-1.0, scalar2=None,
                                          op0=ALU.mult), r=[wqr], w=[wqt])
    P.op("dve", lambda e: e.tensor_copy(out=wqt[:, :, :, 32:64], in_=wqr[:, :, :, 0:32]), r=[wqr, wqt], w=[wqt])

    pS = PS[0:3]
    pO, pD, pQ, pQr, pQt = PS[3], PS[4], PS[5], PS[6], PS[7]
    pending_o = []
    pending_cp = []
    tile_no = 0

    def load_q_inputs(qi):
        cq_, cs_ = cqt[qi % 2], cst[qi % 2]
        for c_ in range(3):
            P.load("sp", cq_, cq_[:, c_, :], Gv[c_][qi // 4, :, (qi % 4) * 512:(qi % 4 + 1) * 512], r=[Gdone[c_]])
        P.load("sp", cs_, cs_[0:64, :, :], cs_d[:, :, qi * 512:(qi + 1) * 512])

    def emit_qproj(h, qi):
        cq_, cs_, qn_, qr_ = cqt[qi % 2], cst[qi % 2], qn[qi % 2], qr[qi % 2]

        def pe_q(e):
            for c in range(3):
                e.matmul(pQ[:, :], lhsT=wqn[:, h, c, :], rhs=cq_[:, c, :], start=(c == 0), stop=(c == 2))
            for c in range(3):
                e.matmul(pQr[0:64, :], lhsT=wqr[:, h, c, :], rhs=cq_[:, c, :], start=(c == 0), stop=(c == 2))
            ins = None
            for c in range(3):
                ins = e.matmul(pQt[0:64, :], lhsT=wqt[:, h, c, :], rhs=cq_[:, c, :], start=(c == 0), stop=(c == 2))
            return ins
        P.op("pe", pe_q, r=[wqn, wqr, wqt, cq_], w=[pQ, pQr, pQt])
        P.op("act", lambda e: e.mul(out=qn_[:], in_=pQ[:, :], mul=ATTN_SCALE), r=[pQ], w=[qn_])
        P.op("dve", lambda e: e.scalar_tensor_tensor(out=t1[0:64, :], in0=pQr[0:64, :], scalar=ATTN_SCALE,
                                                     in1=cs_[0:64, 0, :], op0=ALU.mult, op1=ALU.mult),
             r=[pQr, cs_], w=[t1])
        P.op("dve", lambda e: e.scalar_tensor_tensor(out=t2[0:64, :], in0=pQt[0:64, :], scalar=ATTN_SCALE,
                                                     in1=cs_[0:64, 1, :], op0=ALU.mult, op1=ALU.mult),
             r=[pQt, cs_], w=[t2])
        P.op("dve", lambda e: e.tensor_tensor(out=qr_[0:64, :], in0=t1[0:64, :], in1=t2[0:64, :], op=ALU.add),
             r=[t1, t2], w=[qr_])

    for h in range(2):
        for kt16 in range(16):
            pk = PS[5 + (kt16 % 2)]

            def pe(e, kt16=kt16, pk=pk, h=h):
                ins = None
                for c in range(2):
                    ins = e.matmul(pk[:, :], lhsT=wuk[:, h, c, :], rhs=ckvT[:, c, kt16 * 512:(kt16 + 1) * 512],
                                   start=(c == 0), stop=(c == 1))
                return ins
            P.op("pe", pe, r=[wuk, ckvT], w=[pk])
            if kt16 % 2 == 0:
                P.op("act", lambda e, kt16=kt16, pk=pk: e.copy(out=KT[:, kt16 * 512:(kt16 + 1) * 512], in_=pk[:, :]),
                     r=[pk], w=[KT])
            else:
                P.op("dve", lambda e, kt16=kt16, pk=pk: e.tensor_copy(out=KT[:, kt16 * 512:(kt16 + 1) * 512], in_=pk[:, :]),
                     r=[pk], w=[KT])
        for kg in range(16):
            pv = PS[5 + (kg % 2)]

            def pe(e, kg=kg, pv=pv, h=h):
                ins = None
                for k4 in range(4):
                    kt = kg * 4 + k4
                    for c in range(2):
                        ins = e.matmul(pv[:, k4 * 128:(k4 + 1) * 128], lhsT=ckvT[:, c, kt * 128:(kt + 1) * 128],
                                       rhs=wuv[:, h, c, :], start=(c == 0), stop=(c == 1))
                return ins
            P.op("pe", pe, r=[wuv, ckvT], w=[pv])
            if kg % 2 == 0:
                P.op("act", lambda e, kg=kg, pv=pv: e.copy(out=V[:, kg * 4:(kg + 1) * 4, :],
                                                           in_=pv[:, :].rearrange("p (k d) -> p k d", k=4)), r=[pv], w=[V])
            else:
                P.op("dve", lambda e, kg=kg, pv=pv: e.tensor_copy(out=V[:, kg * 4:(kg + 1) * 4, :],
                                                                  in_=pv[:, :].rearrange("p (k d) -> p k d", k=4)),
                     r=[pv], w=[V])
        for qi in range(NQT):
            while pending_cp and pending_cp[0][0] < tile_no:
                io["emit_o_copy"](pending_cp.pop(0)[1])
            while pending_o and pending_o[0][0] < tile_no:
                q_ = pending_o.pop(0)[1]
                io["emit_o_cc"](q_)
                pending_cp.append((tile_no, q_))
            tile_no += 1
            s2 = qi % 2
            cq_, cs_, qn_, qr_ = cqt[s2], cst[s2], qn[s2], qr[s2]
            if qi == 0:
                load_q_inputs(0)
                load_q_inputs(1)

            if qi == 0:
                emit_qproj(h, 0)
                load_q_inputs(2)
            nk = 4 * (qi + 1)

            def emit_S(kt, qn_=qn_, qr_=qr_):
                ps = pS[kt % 3]

                def pe(e, kt=kt, ps=ps):
                    e.matmul(ps[:, :], lhsT=KT[:, kt * 128:(kt + 1) * 128], rhs=qn_[:, :], start=True, stop=False)
                    return e.matmul(ps[:, :], lhsT=krT[0:64, kt * 128:(kt + 1) * 128], rhs=qr_[0:64, :],
                                    start=False, stop=True)
                P.op("pe", pe, r=[KT, krT, qn_, qr_], w=[ps])

            emit_S(0)
            emit_S(1)
            for kt in range(nk):
                if kt == 0 and qi + 1 < NQT:
                    emit_qproj(h, qi + 1)
                    if qi + 3 < NQT:
                        load_q_inputs(qi + 3)
                if kt + 2 < nk:
                    emit_S(kt + 2)
                ps = pS[kt % 3]
                pt = PT[kt % 3]
                P.op("act", lambda e, ps=ps, pt=pt: e.activation(out=pt[:], in_=ps[:, :], func=AF.Exp), r=[ps], w=[pt])
                if kt >= 4 * qi:
                    j = kt - 4 * qi
                    P.op("dve", lambda e, pt=pt, j=j: e.tensor_tensor(out=pt[:], in0=pt[:], in1=mask[:, j, :], op=ALU.mult),
                         r=[pt, mask], w=[pt])

                def pe_pv(e, kt=kt, pt=pt, nk=nk):
                    e.matmul(pO[:, :], lhsT=V[:, kt, :], rhs=pt[:], start=(kt == 0), stop=(kt == nk - 1))
                    return e.matmul(pD[:, :], lhsT=ones_ap, rhs=pt[:], start=(kt == 0), stop=(kt == nk - 1))
                P.op("pe", pe_pv, r=[V, pt, ones_buf], w=[pO, pD])
            P.op("dve", lambda e: e.reciprocal(out=rden[:], in_=pD[:, :]), r=[pD], w=[rden])
            oo_ = oo[s2]
            P.op("dve", lambda e, oo_=oo_: e.tensor_tensor(out=oo_[:], in0=pO[:, :], in1=rden[:], op=ALU.mult),
                 r=[pO, rden], w=[oo_])
            P.store("sp", OTx[(qi // 4) * 2 + h][:, (qi % 4) * 512:(qi % 4 + 1) * 512], oo_, oo_[:],
                    w=[io["OTx_parts"][h * NQT + qi]])
            if qi % 4 == 3:
                pending_o.append((tile_no, (qi // 4) * 2 + h))
    while pending_cp:
        io["emit_o_copy"](pending_cp.pop(0)[1])
    while pending_o:
        q_ = pending_o.pop(0)[1]
        io["emit_o_cc"](q_)
        io["emit_o_copy"](q_)


def attn_mask_np():
    m = np.zeros((128, 4, 512), np.float32)
    for j in range(4):
        kc = (128 * j + np.arange(128)) // 64
        qc = np.arange(512) // 64
        m[:, j, :] = (kc[:, None] <= qc[None, :]).astype(np.float32)
    return m.astype(NPBF)


def declare_C(nc):
    io = {}
    io["wo"] = nc.dram_tensor("wo", [D, D], F32, kind="ExternalInput")
    io["fg"] = nc.dram_tensor("fg", [128, D], F32, kind="ExternalInput")
    io["W1"] = moe_dram_inputs(nc, "m1_")
    io["out"] = nc.dram_tensor("out", [TPC, D], F32, kind="ExternalOutput")
    io["xs3"] = nc.dram_tensor("xs3", [TPC, D], F32)
    return io


def phase_C(P, nc, PS, C, io):
    x2_d, wo_d, fg_d, W, out_d, xs3 = io["x2"], io["wo"], io["fg"], io["W1"], io["out"], io["xs3"]
    ot_d = io["OTg"]
    epsb = C["epsb"]
    xt = [P.shared(f"xt{s}", [128, D], F32) for s in range(2)]
    xo = [P.shared(f"xo{s}", [128, D], F32) for s in range(2)]
    scr = P.shared("scr", [128, D], F32)
    OTs = P.sb("OTs", [128, 8, TPC], BF16)
    wo = P.sb("wo_s", [128, 8, D], BF16)
    fg = P.sb("fg_s", [128, D], F32)
    fss = P.sb("fss", [128, 1], F32)
    frs = P.sb("frs", [128, 1], F32)
    fo = [P.sb(f"fo{s}", [128, D], F32) for s in range(2)]
    otidx = P.sb("otidx_s", [128, 8], I32)
    P.load("sp", otidx, otidx[:], io["otidx"][:, :])
    for k in range(8):
        P.dma("pool", lambda e, k=k: e.indirect_dma_start(
            out=OTs[:, k, :], out_offset=None, in_=ot_d[:, :],
            in_offset=bass.IndirectOffsetOnAxis(ap=otidx[:, k:k + 1], axis=0)),
            r=list(io["OTg_done"]) + [otidx], w=[OTs], lane="otg_gather")
    P.load("sp", fg, fg[:], fg_d[:, :])
    P.dma("pool", lambda e: e.dma_start(out=wo[:], in_=wo_d.ap().rearrange("(k p) f -> p k f", p=128)), w=[wo], lane="wo")
    xs3_parts = [Tn(None, f"xs3p{i}") for i in range(NT)]
    P.load("sp", xt[0], xt[0][:], x2_d[0:128, :], r=io["x2_parts"])
    for i in range(NT):
        s = i % 2
        x_, xo_ = xt[s], xo[s]
        if i + 1 < NT:
            xn = xt[(i + 1) % 2]
            P.load("sp", xn, xn[:], x2_d[(i + 1) * 128:(i + 2) * 128, :], r=io["x2_parts"])
        for dh in range(2):
            pa = PS[dh]

            def pe(e, dh=dh, pa=pa, i=i):
                ins = None
                for k in range(8):
                    ins = e.matmul(pa[:, :], lhsT=OTs[:, k, i * 128:(i + 1) * 128], rhs=wo[:, k, dh * 512:(dh + 1) * 512],
                                   start=(k == 0), stop=(k == 7))
                return ins
            P.op("pe", pe, r=[OTs, wo], w=[pa])
            P.op("dve", lambda e, dh=dh, pa=pa, x_=x_, xo_=xo_: e.tensor_tensor(
                out=xo_[:, dh * 512:(dh + 1) * 512], in0=pa[:, :], in1=x_[:, dh * 512:(dh + 1) * 512], op=ALU.add),
                r=[pa, x_, xo_], w=[xo_])
        P.store("sp", xs3[i * 128:(i + 1) * 128, :], xo_, xo_[:], w=[xs3_parts[i]])

    def post(i, xo_):
        emit_rstd(P, (xo_[:], xo_), scr, fss, frs, epsb, D)
        fo_ = fo[i % 2]
        P.op("dve", lambda e: e.scalar_tensor_tensor(out=fo_[:], in0=xo_[:], scalar=frs[:, 0:1], in1=fg[:],
                                                     op0=ALU.mult, op1=ALU.mult), r=[xo_, frs, fg], w=[fo_])
        P.store("sp", out_d[i * 128:(i + 1) * 128, :], fo_, fo_[:], final=True)

    moe_layer(P, "m1_", xs3, xs3_parts, PS, C, W, post)


GROUPS = [[0, 1, 2, 3], [4, 5, 6, 7]]


def build_F():
    nc = bass.Bass("TRN2", target_bir_lowering=False)
    P = Prog(nc)
    io = declare_A(nc)
    io.update(declare_B(nc))
    io.update(declare_C(nc))
    PS = [P.psum(f"ps{b}", [128, 512], F32) for b in range(8)]
    C = load_consts(P, io["cb"], io["cf"])
    mark = P.mark()
    phase_A_pool(P, nc, PS, C, io)
    P.release(mark)
    phase_A_moe(P, nc, PS, C, io)
    io["G_done"] = [Tn(None, f"G_done{k}") for k in range(6)]
    for k in range(6):
        P.dma("pool", lambda e, k=k: e.collective_compute("AllGather", ALU.bypass, replica_groups=GROUPS,
                                                          ins=[io["latT"][k].ap().opt()], outs=[io["G"][k].ap().opt()]),
              r=[io["latT_done"][k]], w=[io["G_done"][k]], lane=f"cc_ag{k}", inc=1)
    P.release(mark)
    io["OTx_parts"] = [Tn(None, f"otx{i}") for i in range(2 * NQT)]
    io["OTg_done"] = [Tn(None, f"OTg_done{q}") for q in range(8)]

    def emit_o_cc(q):
        j_, hh_ = q // 2, q % 2
        parts = [io["OTx_parts"][hh_ * NQT + qi] for qi in range(4 * j_, 4 * j_ + 4)]
        P.dma("pool", lambda e, q=q: e.collective_compute("AllGather", ALU.bypass, replica_groups=GROUPS,
                                                          ins=[io["OTx"][q].ap().opt()],
                                                          outs=[io["OTg"][q * 512:(q + 1) * 512, :].opt()]),
              r=parts, w=[io["OTg_done"][q]], lane=f"cc_o{q}", inc=1)

    def emit_o_copy(q):
        pass

    io["emit_o_cc"] = emit_o_cc
    io["emit_o_copy"] = emit_o_copy
    phase_B(P, nc, PS, C, io)
    P.release(mark)
    phase_C(P, nc, PS, C, io)
    P.finish()
    return nc


def kernel(**inputs):
    inp = {k: np.asarray(v) for k, v in inputs.items()}
    if "F" not in _CACHE:
        _CACHE["F"] = build_F()
    nc = _CACHE["F"]
    x = inp["x"].reshape(NCORES, TPC, D)
    cb, cf = host_consts()
    cosf, sinf = rope_np()
    bc = lambda v: np.ascontiguousarray(np.broadcast_to(v[None, :], (128, v.shape[0])))
    pvec = np.stack([bc(inp["pool_norm"][0]), bc(inp["pool_b"][0].reshape(-1)), bc(inp["pool_scale"][0])])
    lvec = np.stack([bc(inp["kv_in_norm"]), bc(inp["attn_norm"][0])])
    lvec2 = np.ascontiguousarray(np.concatenate([bc(inp["kv_norm"]), bc(inp["q_norm"][0])], axis=1))
    csT = np.ascontiguousarray(np.stack([np.concatenate([cosf.T, cosf.T], axis=0),
                                         np.concatenate([sinf.T, sinf.T], axis=0)], axis=1))
    wq = inp["wq_up"][0].reshape(384, 8, 192)
    wuk = inp["w_uk"].reshape(256, 8, 128)
    wuv = inp["w_uv"].reshape(256, 8, 128)
    common = {
        "cb": cb, "cf": cf, "pvec": pvec, "pool_w": inp["pool_w"][0], "lvec": lvec, "lvec2": lvec2,
        "w_dkv": inp["w_dkv"], "wq_down": inp["wq_down"][0], "csT": csT, "mask": attn_mask_np(),
        "wo": inp["wo"][0], "fg": bc(inp["final_norm"]),
    }
    common.update(moe_host_inputs("m0_", 0, inp))
    common.update(moe_host_inputs("m1_", 1, inp))
    in_maps = []
    for c in range(NCORES):
        m_ = c % 4
        halo = np.zeros((128, D), np.float32) if m_ == 0 else x[c - 1, -128:, :]
        am, ah, ic = pool_consts(c)
        p0 = m_ * TPC
        cs = np.concatenate([cosf[p0:p0 + TPC], sinf[p0:p0 + TPC]], axis=1)
        cs = np.ascontiguousarray(cs.reshape(NT, 128, 64).transpose(1, 0, 2))
        hs = [2 * m_, 2 * m_ + 1]
        m = dict(common)
        m.update({
            "xin": np.ascontiguousarray(np.concatenate([halo, x[c]], axis=0)), "amain": am, "ahalo": ah, "invc": ic, "cs": cs,
            "wqn": np.ascontiguousarray(np.stack([wq[:, h, 0:128] for h in hs])),
            "wqr": np.ascontiguousarray(np.stack([wq[:, h, 128:192] for h in hs])),
            "wuk": np.ascontiguousarray(np.stack([wuk[:, h, :] for h in hs])),
            "wuv": np.ascontiguousarray(np.stack([wuv[:, h, :] for h in hs])),
            "otidx": np.ascontiguousarray(np.stack(
                [(m_ * 2 + (k % 2)) * 512 + (k // 2) * 128 + np.arange(128) for k in range(8)], axis=1).astype(np.int32)),
        })
        in_maps.append(m)
    res = run_bass_kernel_spmd(nc, in_maps, core_ids=list(range(NCORES)))
    out = np.stack([np.asarray(r["out"]) for r in res.results])
    return out.reshape(2, SEQ, D).astype(np.float32)
```

```python
import numpy as np
import ml_dtypes
import concourse.bass as bass
import concourse.mybir as mybir
from concourse.bass_utils import run_bass_kernel_spmd

F32 = mybir.dt.float32
BF16 = mybir.dt.bfloat16
I32 = mybir.dt.int32
ALU = mybir.AluOpType
AF = mybir.ActivationFunctionType
AX = mybir.AxisListType
NPBF = ml_dtypes.bfloat16

NCORES = 8
D = 1024
SEQ = 8192
TPC = 2048
NT = TPC // 128
E = 32
CAP = 256
NSLOT = E * CAP
EPS = 1e-6
ATTN_SCALE = 1.0 / float(np.sqrt(192.0))
BIG = 1.0e4


class Tn:
    def __init__(self, h, name):
        self.h = h
        self.name = name
        self.w = []
        self.r = []

    def __getitem__(self, k):
        return self.h[k]


class Prog:
    ENG = ("pe", "act", "dve", "pool", "sp")

    def __init__(self, nc):
        self.nc = nc
        self.q = {k: [] for k in self.ENG}
        self.esem = {k: nc.alloc_semaphore("es_" + k) for k in ("pe", "act", "dve", "pool")}
        self.ecnt = {k: 0 for k in self.esem}
        self.seen = {k: {} for k in self.ENG}
        self.lanes = {}
        self.free_lanes = []
        self.nlane = 0
        self.final = []
        self.phase = 0
        self._sb_off = 16512
        self._sb_top = 229344
        self._shared = {}

    def sb(self, name, shape, dt):
        nb = 1
        for d_ in shape[1:]:
            nb *= d_
        nb *= {F32: 4, BF16: 2, I32: 4}[dt]
        off = (self._sb_off + 63) // 64 * 64
        assert off + nb <= self._sb_top, f"SBUF arena overflow at {name}: {off}+{nb}"
        h = self.nc.alloc_sbuf_tensor_at(f"s{self.phase}_" + name, list(shape), dt, offset=off)
        self._sb_off = off + nb
        return Tn(h, name)

    def mark(self):
        return self._sb_off

    def release(self, mark):
        self.barrier()
        self._sb_off = mark
        self._shared = {}
        self.phase += 1

    def barrier(self):
        toks = [(self.esem[k], "es_" + k, self.ecnt[k]) for k in self.esem]
        toks += [(L[0], L[2], L[1]) for L in self.lanes.values()]
        for eng in self.ENG:
            seen = self.seen[eng]
            waits = []
            for (sem, key, val) in toks:
                if val <= 0 or seen.get(key, -1) >= val:
                    continue
                seen[key] = val
                waits.append((sem, val))

            def emit(e, waits=waits):
                for (s_, v) in waits:
                    e.wait_ge(s_, v)
            self.q[eng].append(emit)
        self.free_lanes += [L for L in self.lanes.values() if L[3] != "pool" and L[1] % 16 == 0]
        self.lanes = {}

    def bc_reg(self, e):
        if getattr(self, "_bc", None) is None:
            self._bc = e.to_reg(NSLOT - 1)
        return self._bc

    def shared(self, name, shape, dt):
        if name not in self._shared:
            self._shared[name] = (self.sb(name, shape, dt), list(shape), dt)
        t, sh, d = self._shared[name]
        assert sh == list(shape) and d == dt, name
        return t

    def psum(self, name, shape, dt=F32):
        return Tn(self.nc.alloc_psum_tensor("p_" + name, list(shape), dt), name)

    def dram(self, name, shape, dt, kind="Internal"):
        return Tn(self.nc.dram_tensor(f"d{self.phase}_" + name, list(shape), dt, kind=kind), name)

    def _waits(self, eng, r, w):
        toks = []
        for b in r:
            toks += b.w
        for b in w:
            toks += b.w
            toks += b.r
        out = []
        seen = self.seen[eng]
        for (sem, key, val) in toks:
            if eng == "pe" and key == "es_pe":
                continue
            if seen.get(key, -1) >= val:
                continue
            seen[key] = val
            out.append((sem, val))
        return out

    def _commit(self, tok, r, w):
        for b in r:
            b.r.append(tok)
        for b in w:
            b.w = [tok]
            b.r = []

    def op(self, eng, fn, r=(), w=()):
        waits = self._waits(eng, r, w)
        self.ecnt[eng] += 1
        sem = self.esem[eng]
        tok = (sem, "es_" + eng, self.ecnt[eng])

        def emit(e, waits=waits, fn=fn, sem=sem):
            for (s, v) in waits:
                e.wait_ge(s, v)
            fn(e).then_inc(sem, 1)

        self.q[eng].append(emit)
        self._commit(tok, r, w)
        return tok

    def dma(self, q, fn, r=(), w=(), lane=None, final=False, inc=16):
        waits = self._waits(q, r, w)
        if lane not in self.lanes:
            if q != "pool" and self.free_lanes:
                self.lanes[lane] = self.free_lanes.pop()
            else:
                self.nlane += 1
                self.lanes[lane] = [self.nc.alloc_semaphore(f"ln{self.nlane}"), 0, f"ln{self.nlane}", q]
        assert self.lanes[lane][3] == q, f"lane {lane} used from two queues"
        L = self.lanes[lane]
        L[1] += inc
        sem = L[0]
        tok = (sem, L[2], L[1])

        def emit(e, waits=waits, fn=fn, sem=sem, inc=inc):
            for (s, v) in waits:
                e.wait_ge(s, v)
            fn(e).then_inc(sem, inc)

        self.q[q].append(emit)
        self._commit(tok, r, w)
        if final:
            self.final.append(tok)
        return tok

    def load(self, q, out_t, out_ap, in_ap, r=(), lane=None):
        return self.dma(q, lambda e: e.dma_start(out=out_ap, in_=in_ap), r=r, w=[out_t],
                        lane=lane or out_t.name)

    def store(self, q, out_ap, in_t, in_ap, w=(), lane=None, final=False):
        return self.dma(q, lambda e: e.dma_start(out=out_ap, in_=in_ap), r=[in_t], w=w,
                        lane=lane or in_t.name, final=final)

    def finish(self):
        waits = []
        seen = self.seen["sp"]
        for (sem, key, val) in self.final:
            if seen.get(key, -1) >= val:
                continue
            seen[key] = val
            waits.append((sem, val))

        def emit(e, waits=waits):
            for (s, v) in waits:
                e.wait_ge(s, v)

        self.q["sp"].append(emit)
        q = self.q
        with self.nc.Block() as blk:
            @blk.sync
            def _(e):
                for f in q["sp"]:
                    f(e)

            @blk.tensor
            def _(e):
                for f in q["pe"]:
                    f(e)

            @blk.scalar
            def _(e):
                for f in q["act"]:
                    f(e)

            @blk.vector
            def _(e):
                for f in q["dve"]:
                    f(e)

            @blk.gpsimd
            def _(e):
                for f in q["pool"]:
                    f(e)


def emit_rstd(P, x_t, scr_t, ss_t, rs_t, epsb, n, eng_sq="act"):
    x_ap, x_buf = x_t
    P.op("act", lambda e: e.activation(out=scr_t[:, 0:n], in_=x_ap, func=AF.Square, accum_out=ss_t[:, 0:1]),
         r=[x_buf], w=[scr_t, ss_t])
    P.op("act", lambda e: e.activation(out=ss_t[:, 0:1], in_=ss_t[:, 0:1], func=AF.Sqrt, scale=1.0 / n,
                                       bias=epsb[:, 0:1]), r=[ss_t, epsb], w=[ss_t])
    P.op("dve", lambda e: e.reciprocal(out=rs_t[:, 0:1], in_=ss_t[:, 0:1]), r=[ss_t], w=[rs_t])


def emit_transposes(P, src_t, dst_t, ps_t, ident_ap, ident_buf, n, evac="dve", dst_off=0, width=128):
    psb = ps_t[:, :].bitcast(BF16)

    def pe(e):
        ins = None
        for k in range(n):
            ins = e.transpose(out=psb[:, k * 128:(k + 1) * 128], in_=src_t[:, k * 128:(k + 1) * 128],
                              identity=ident_ap)
        return ins

    P.op("pe", pe, r=[src_t, ident_buf], w=[ps_t])
    src_v = psb[:, 0:n * 128].rearrange("p (k t) -> p k t", k=n)
    if evac == "dve":
        P.op("dve", lambda e: e.tensor_copy(out=dst_t[:, 0:n, dst_off:dst_off + 128], in_=src_v), r=[ps_t], w=[dst_t])
    else:
        P.op("act", lambda e: e.copy(out=dst_t[:, 0:n, dst_off:dst_off + 128], in_=src_v), r=[ps_t], w=[dst_t])


def moe_layer(P, pfx, xs, xs_deps, PS, C, W, post):
    nc = P.nc
    n = pfx
    xt = [P.shared(f"xt{s}", [128, D], F32) for s in range(2)]
    hb = [P.shared(f"hb{s}", [128, D], BF16) for s in range(2)]
    scr = P.shared("scr", [128, D], F32)
    ss = P.sb(f"{n}ss", [128, 1], F32)
    rstd = P.sb(f"{n}rstd", [128, 1], F32)
    gbc = P.sb(f"{n}gbc", [128, D], F32)
    gfm = P.sb(f"{n}gfm", [128, 8], F32)
    wr = P.sb(f"{n}wr", [128, 8, 36], F32)
    brbc = P.sb(f"{n}brbc", [128, 36], F32)
    xT = P.sb(f"{n}xT", [128, 8, 128], F32)
    L = P.sb(f"{n}L", [128, 36], F32)
    sm = P.sb(f"{n}sm", [128, 16], F32)
    ohg = P.sb(f"{n}ohg", [128, 4], F32)
    eg = P.sb(f"{n}eg", [128, 4], F32)
    pen = P.sb(f"{n}pen", [128, 4], F32)
    Lm = P.sb(f"{n}Lm", [128, 32], F32)
    Lm2 = P.sb(f"{n}Lm2", [128, 32], F32)
    oh = [P.sb(f"{n}oh{k}", [128, 32], F32) for k in range(2)]
    ind = P.sb(f"{n}ind", [128, 32], BF16)
    cnt = P.sb(f"{n}cnt", [128, 32], F32)
    posv = P.sb(f"{n}posv", [128, 32], F32)
    ovf = P.sb(f"{n}ovf", [128, 32], F32)
    tmp32 = P.sb(f"{n}tmp32", [128, 32], F32)
    slotf = P.sb(f"{n}slotf", [128, 2], F32)
    gclf = P.sb(f"{n}gclf", [128, 2], F32)
    val = P.sb(f"{n}val", [128, 2], F32)
    slot_i = [[P.sb(f"{n}sl{i}_{k}", [128, 1], I32) for k in range(2)] for i in range(NT)]
    gidx_i = [[P.sb(f"{n}gi{i}_{k}", [128, 1], I32) for k in range(2)] for i in range(NT)]
    gates = [P.sb(f"{n}gt{i}", [128, 2], F32) for i in range(NT)]
    NWS = 3
    wg = [P.sb(f"{n}wg{s}", [128, 8, 512], BF16) for s in range(NWS)]
    wu = [P.sb(f"{n}wu{s}", [128, 8, 512], BF16) for s in range(NWS)]
    wd = [P.sb(f"{n}wd{s}", [128, 4, 1024], BF16) for s in range(NWS)]
    xe = P.sb(f"{n}xe", [128, 2, D], BF16)
    xeT = P.sb(f"{n}xeT", [128, 8, 256], BF16)
    HT = P.sb(f"{n}HT", [128, 4, 256], BF16)
    sg = P.shared("sg", [128, D], F32)
    yo = [P.sb(f"{n}yo{j}", [128, D], F32) for j in range(2)]
    y01 = [P.sb(f"{n}yg{k}", [128, D], F32) for k in range(2)]
    xo = [P.shared(f"xo{s}", [128, D], F32) for s in range(2)]
    Xd = P.dram(f"{n}Xd", [NSLOT, D], BF16)
    Yd = P.dram(f"{n}Yd", [NSLOT, D], F32)
    Xd_parts = [Tn(None, f"{n}Xdp{j}") for j in range(2 * NT)]
    zt = P.sb(f"{n}zt", [128, D], BF16)
    Xd_zero = Tn(None, f"{n}Xd_zero")
    P.op("pool", lambda e: e.memset(zt[:], 0.0), w=[zt])
    nzb = NSLOT // 128
    for zb in range(nzb):
        P.dma("sp", lambda e, zb=zb: e.dma_start(
            out=Xd[zb * 128:(zb + 1) * 128, :], in_=zt[:]),
            r=[zt], w=[Xd_zero] if zb == nzb - 1 else [], lane=f"{n}zfill")
    Yd_parts = [Tn(None, f"{n}Ydp{e}") for e in range(2 * E)]

    ident_b, U_b, ones_b, ident_f, ebase, epsb = C["ident_b"], C["U_b"], C["ones_b"], C["ident_f"], C["ebase"], C["epsb"]
    cb, cf = C["cb"], C["cf"]

    def load_expert_gu(e_):
        s = e_ % NWS
        P.dma("pool", lambda e: e.dma_start(out=wg[s][:], in_=W["w_gate"][e_].rearrange("(k p) f -> p k f", p=128)),
              w=[wg[s]], lane=wg[s].name)
        P.dma("pool", lambda e: e.dma_start(out=wu[s][:], in_=W["w_up"][e_].rearrange("(k p) f -> p k f", p=128)),
              w=[wu[s]], lane=wu[s].name)

    def load_expert_d(e_):
        s = e_ % NWS
        P.dma("pool", lambda e: e.dma_start(out=wd[s][:], in_=W["w_down"][e_].rearrange("(k p) f -> p k f", p=128)),
              w=[wd[s]], lane=wd[s].name)

    def load_expert(e_):
        load_expert_gu(e_)
        load_expert_d(e_)

    for e0 in range(NWS):
        load_expert(e0)

    P.load("sp", gbc, gbc[:], W["g_bc"][:, :])
    P.load("sp", gfm, gfm[:], W["g_fm"][:, :])
    P.load("sp", wr, wr[:], W["wr"].rearrange("(k p) f -> p k f", p=128))
    P.load("sp", brbc, brbc[:], W["br_bc"][:, :])
    for k in range(8):
        P.op("dve", lambda e, k=k: e.tensor_scalar(out=wr[:, k, :], in0=wr[:, k, :], scalar1=gfm[:, k:k + 1],
                                                   scalar2=None, op0=ALU.mult), r=[wr, gfm], w=[wr])
    P.op("dve", lambda e: e.memset(cnt[:], 0.0), w=[cnt])

    pTa, pTb, pL, pPos, pCnt = PS[0], PS[1], PS[2], PS[3], PS[4]

    for i in range(NT):
        s = i % 2
        x_ = xt[s]
        h_ = hb[s]
        P.load("sp", x_, x_[:], xs[i * 128:(i + 1) * 128, :], r=xs_deps)
        emit_rstd(P, (x_[:], x_), scr, ss, rstd, epsb, D)
        P.op("dve", lambda e, x_=x_, h_=h_: e.scalar_tensor_tensor(out=h_[:], in0=x_[:], scalar=rstd[:, 0:1], in1=gbc[:],
                                                                     op0=ALU.mult, op1=ALU.mult),
             r=[x_, rstd, gbc], w=[h_])
        for half, pt in ((0, pTa), (1, pTb)):
            def pe(e, half=half, pt=pt, x_=x_):
                ins = None
                for kk in range(4):
                    k = half * 4 + kk
                    ins = e.transpose(out=pt[:, kk * 128:(kk + 1) * 128], in_=x_[:, k * 128:(k + 1) * 128],
                                      identity=ident_f)
                return ins
            P.op("pe", pe, r=[x_, cf], w=[pt])
            P.op("act", lambda e, half=half, pt=pt: e.copy(out=xT[:, half * 4:(half + 1) * 4, :],
                                                           in_=pt[:, :].rearrange("p (k t) -> p k t", k=4)),
                 r=[pt], w=[xT])

        def pe_logits(e):
            ins = None
            for k in range(8):
                ins = e.matmul(pL[:, 0:36], lhsT=xT[:, k, :], rhs=wr[:, k, :], start=(k == 0), stop=(k == 7))
            return ins
        P.op("pe", pe_logits, r=[xT, wr], w=[pL])
        P.op("dve", lambda e: e.scalar_tensor_tensor(out=L[:], in0=pL[:, 0:36], scalar=rstd[:, 0:1], in1=brbc[:],
                                                     op0=ALU.mult, op1=ALU.add), r=[pL, rstd, brbc], w=[L])
        P.op("dve", lambda e: e.reduce_max(out=sm[:, 0:1], in_=L[:, 0:4], axis=AX.X), r=[L], w=[sm])
        P.op("dve", lambda e: e.tensor_scalar(out=ohg[:], in0=L[:, 0:4], scalar1=sm[:, 0:1], scalar2=None,
                                              op0=ALU.is_equal), r=[L, sm], w=[ohg])
        P.op("dve", lambda e: e.tensor_scalar(out=sm[:, 1:2], in0=sm[:, 0:1], scalar1=-1.0, scalar2=None,
                                              op0=ALU.mult), r=[sm], w=[sm])
        P.op("act", lambda e: e.activation(out=eg[:], in_=L[:, 0:4], func=AF.Exp, bias=sm[:, 1:2], scale=1.0,
                                           accum_out=sm[:, 2:3]), r=[L, sm], w=[eg, sm])
        P.op("dve", lambda e: e.reciprocal(out=sm[:, 3:4], in_=sm[:, 2:3]), r=[sm], w=[sm])
        P.op("dve", lambda e: e.tensor_scalar(out=pen[:], in0=ohg[:], scalar1=BIG, scalar2=-BIG, op0=ALU.mult,
                                              op1=ALU.add), r=[ohg], w=[pen])

        def masklog(e):
            ins = None
            for g in range(4):
                ins = e.tensor_scalar(out=Lm[:, g * 8:(g + 1) * 8], in0=L[:, 4 + g * 8:12 + g * 8],
                                      scalar1=pen[:, g:g + 1], scalar2=None, op0=ALU.add)
            return ins
        P.op("dve", masklog, r=[L, pen], w=[Lm])
        P.op("dve", lambda e: e.reduce_max(out=sm[:, 4:5], in_=Lm[:], axis=AX.X), r=[Lm], w=[sm])
        P.op("dve", lambda e: e.tensor_scalar(out=oh[0][:], in0=Lm[:], scalar1=sm[:, 4:5], scalar2=None,
                                              op0=ALU.is_equal), r=[Lm, sm], w=[oh[0]])
        P.op("dve", lambda e: e.scalar_tensor_tensor(out=Lm2[:], in0=oh[0][:], scalar=-BIG, in1=Lm[:],
                                                     op0=ALU.mult, op1=ALU.add), r=[oh[0], Lm], w=[Lm2])
        P.op("dve", lambda e: e.reduce_max(out=sm[:, 5:6], in_=Lm2[:], axis=AX.X), r=[Lm2], w=[sm])
        P.op("dve", lambda e: e.tensor_scalar(out=oh[1][:], in0=Lm2[:], scalar1=sm[:, 5:6], scalar2=None,
                                              op0=ALU.is_equal), r=[Lm2, sm], w=[oh[1]])
        P.op("dve", lambda e: e.tensor_tensor(out=sm[:, 6:7], in0=sm[:, 5:6], in1=sm[:, 4:5], op=ALU.subtract),
             r=[sm], w=[sm])
        P.op("act", lambda e: e.activation(out=sm[:, 7:8], in_=sm[:, 6:7], func=AF.Exp), r=[sm], w=[sm])
        P.op("dve", lambda e: e.tensor_scalar(out=sm[:, 8:9], in0=sm[:, 7:8], scalar1=1.0, scalar2=None,
                                              op0=ALU.add), r=[sm], w=[sm])
        P.op("dve", lambda e: e.reciprocal(out=sm[:, 9:10], in_=sm[:, 8:9]), r=[sm], w=[sm])
        gt = gates[i]
        P.op("dve", lambda e, gt=gt: e.tensor_tensor(out=gt[:, 0:1], in0=sm[:, 3:4], in1=sm[:, 9:10], op=ALU.mult),
             r=[sm], w=[gt])
        P.op("dve", lambda e, gt=gt: e.tensor_tensor(out=gt[:, 1:2], in0=gt[:, 0:1], in1=sm[:, 7:8], op=ALU.mult),
             r=[sm, gt], w=[gt])
        P.op("dve", lambda e: e.tensor_tensor(out=ind[:], in0=oh[0][:], in1=oh[1][:], op=ALU.add),
             r=[oh[0], oh[1]], w=[ind])
        P.op("pe", lambda e: e.matmul(pPos[:, 0:32], lhsT=U_b, rhs=ind[:], start=True, stop=True),
             r=[ind, cb], w=[pPos])
        P.op("pe", lambda e: e.matmul(pCnt[:, 0:32], lhsT=ones_b, rhs=ind[:], start=True, stop=True),
             r=[ind, cb], w=[pCnt])
        P.op("dve", lambda e: e.tensor_tensor(out=posv[:], in0=pPos[:, 0:32], in1=cnt[:], op=ALU.add),
             r=[pPos, cnt], w=[posv])
        P.op("dve", lambda e: e.tensor_tensor(out=cnt[:], in0=pCnt[:, 0:32], in1=cnt[:], op=ALU.add),
             r=[pCnt, cnt], w=[cnt])
        P.op("dve", lambda e: e.tensor_scalar(out=ovf[:], in0=posv[:], scalar1=float(CAP), scalar2=1.0e6,
                                              op0=ALU.is_ge, op1=ALU.mult), r=[posv], w=[ovf])
        P.op("dve", lambda e: e.tensor_tensor(out=posv[:], in0=posv[:], in1=ebase, op=ALU.add), r=[posv, cf], w=[posv])
        P.op("dve", lambda e: e.tensor_tensor(out=posv[:], in0=posv[:], in1=ovf[:], op=ALU.add), r=[posv, ovf], w=[posv])
        for k in range(2):
            P.op("dve", lambda e, k=k: e.tensor_tensor(out=tmp32[:], in0=oh[k][:], in1=posv[:], op=ALU.mult),
                 r=[oh[k], posv], w=[tmp32])
            P.op("dve", lambda e, k=k: e.reduce_sum(out=slotf[:, k:k + 1], in_=tmp32[:], axis=AX.X),
                 r=[tmp32], w=[slotf])
        P.op("dve", lambda e: e.tensor_scalar(out=val[:], in0=slotf[:], scalar1=1.0e5, scalar2=None, op0=ALU.is_lt),
             r=[slotf], w=[val])
        P.op("dve", lambda e, gt=gt: e.tensor_tensor(out=gt[:], in0=gt[:], in1=val[:], op=ALU.mult), r=[gt, val], w=[gt])
        P.op("dve", lambda e: e.tensor_scalar(out=gclf[:], in0=slotf[:], scalar1=float(NSLOT - 1), scalar2=None,
                                              op0=ALU.min), r=[slotf], w=[gclf])
        for k in range(2):
            si = slot_i[i][k]
            gi = gidx_i[i][k]
            P.op("dve", lambda e, k=k, si=si: e.tensor_copy(out=si[:], in_=slotf[:, k:k + 1]), r=[slotf], w=[si])
            P.op("dve", lambda e, k=k, gi=gi: e.tensor_copy(out=gi[:], in_=gclf[:, k:k + 1]), r=[gclf], w=[gi])
            xp = Xd_parts[2 * i + k]
            P.dma("pool", lambda e, si=si, h_=h_: e.indirect_dma_start(
                out=Xd[:, :], out_offset=bass.IndirectOffsetOnAxis(ap=si[:, :], axis=0),
                in_=h_[:, :], in_offset=None, bounds_check=P.bc_reg(e), oob_is_err=False),
                r=[h_, si, Xd_zero], w=[xp], lane=f"{n}sc{s}_{k}")

    pT, pG0, pG1, pU0, pU1, pY0, pY1 = PS[0], PS[1], PS[2], PS[3], PS[4], PS[5], PS[6]
    pTj = (PS[0], PS[7])

    def emit_T_pe(e_):
        P.dma("sp", lambda e, e_=e_: e.dma_start(out=xe[:], in_=Xd[e_ * CAP:(e_ + 1) * CAP, :].rearrange(
            "(j p) c -> p j c", p=128)), r=Xd_parts, w=[xe], lane=xe.name)
        for j in range(2):
            pt_ = pTj[j]
            psb = pt_[:, :].bitcast(BF16)

            def pe(e, j=j, psb=psb):
                ins = None
                for k in range(8):
                    ins = e.transpose(out=psb[:, k * 128:(k + 1) * 128], in_=xe[:, j, k * 128:(k + 1) * 128],
                                      identity=ident_b)
                return ins
            P.op("pe", pe, r=[xe, cb], w=[pt_])

    def emit_T_copy(e_):
        for j in range(2):
            psb = pTj[j][:, :].bitcast(BF16)
            P.op("dve", lambda e, j=j, psb=psb: e.tensor_copy(out=xeT[:, :, j * 128:(j + 1) * 128],
                                                             in_=psb.rearrange("p (k t) -> p k t", k=8)),
                 r=[pTj[j]], w=[xeT])

    emit_T_pe(0)
    emit_T_copy(0)
    for e_ in range(E):
        s = e_ % NWS
        for (wt, p0, p1) in ((wg[s], pG0, pG1), (wu[s], pU0, pU1)):
            for f in range(4):
                pt = p0 if f < 2 else p1

                def pe(e, wt=wt, f=f, pt=pt):
                    ins = None
                    for k in range(8):
                        ins = e.matmul(pt[:, (f % 2) * 256:(f % 2 + 1) * 256], lhsT=wt[:, k, f * 128:(f + 1) * 128],
                                       rhs=xeT[:, k, :], start=(k == 0), stop=(k == 7))
                    return ins
                P.op("pe", pe, r=[wt, xeT], w=[pt])
        if e_ + NWS < E:
            load_expert_gu(e_ + NWS)
        if e_ + 1 < E:
            emit_T_pe(e_ + 1)
        for half, (pg, pu) in enumerate(((pG0, pU0), (pG1, pU1))):
            P.op("act", lambda e, half=half, pg=pg: e.activation(
                out=sg[:, half * 512:(half + 1) * 512], in_=pg[:, :], func=AF.Silu),
                r=[pg], w=[sg])
            P.op("dve", lambda e, half=half, pu=pu: e.tensor_tensor(
                out=HT[:, half * 2:(half + 1) * 2, :], in0=sg[:, half * 512:(half + 1) * 512].rearrange("p (f r) -> p f r", f=2),
                in1=pu[:, :].rearrange("p (f r) -> p f r", f=2), op=ALU.mult), r=[sg, pu], w=[HT])
        if e_ + 1 < E:
            emit_T_copy(e_ + 1)
        for j in range(2):
            for dh, py in ((0, pY0), (1, pY1)):
                def pe(e, j=j, dh=dh, py=py, s=s):
                    ins = None
                    for f in range(4):
                        ins = e.matmul(py[:, :], lhsT=HT[:, f, j * 128:(j + 1) * 128],
                                       rhs=wd[s][:, f, dh * 512:(dh + 1) * 512], start=(f == 0), stop=(f == 3))
                    return ins
                P.op("pe", pe, r=[HT, wd[s]], w=[py])
                if dh == 0:
                    P.op("act", lambda e, j=j, py=py: e.copy(out=yo[j][:, 0:512], in_=py[:, :]), r=[py], w=[yo[j]])
                else:
                    P.op("dve", lambda e, j=j, py=py: e.tensor_copy(out=yo[j][:, 512:1024], in_=py[:, :]),
                         r=[py, yo[j]], w=[yo[j]])
            P.dma("sp", lambda e, j=j, e_=e_: e.dma_start(out=Yd[e_ * CAP + j * 128:e_ * CAP + (j + 1) * 128, :],
                                                         in_=yo[j][:]), r=[yo[j]], w=[Yd_parts[2 * e_ + j]],
                  lane=yo[j].name)
        if e_ + NWS < E:
            load_expert_d(e_ + NWS)
    P.load("sp", xt[0], xt[0][:], xs[0:128, :], r=xs_deps)
    for i in range(NT):
        s = i % 2
        x_ = xt[s]
        if i + 1 < NT:
            xn = xt[(i + 1) % 2]
            P.load("sp", xn, xn[:], xs[(i + 1) * 128:(i + 2) * 128, :], r=xs_deps)
        for k in range(2):
            gi = gidx_i[i][k]
            P.dma("pool", lambda e, k=k, gi=gi: e.indirect_dma_start(
                out=y01[k][:, :], out_offset=None, in_=Yd[:, :],
                in_offset=bass.IndirectOffsetOnAxis(ap=gi[:, :], axis=0), bounds_check=P.bc_reg(e), oob_is_err=False),
                r=Yd_parts + [gi], w=[y01[k]], lane=y01[k].name)
        gt = gates[i]
        xo_ = xo[s]
        P.op("dve", lambda e, x_=x_, gt=gt, xo_=xo_: e.scalar_tensor_tensor(
            out=xo_[:], in0=y01[0][:], scalar=gt[:, 0:1], in1=x_[:], op0=ALU.mult, op1=ALU.add),
            r=[y01[0], gt, x_], w=[xo_])
        P.op("dve", lambda e, gt=gt, xo_=xo_: e.scalar_tensor_tensor(
            out=xo_[:], in0=y01[1][:], scalar=gt[:, 1:2], in1=xo_[:], op0=ALU.mult, op1=ALU.add),
            r=[y01[1], gt, xo_], w=[xo_])
        post(i, xo_)


def load_consts(P, cb_d, cf_d):
    cb = P.sb("cb_s", [128, 3 * 128], BF16)
    cf = P.sb("cf_s", [128, 128 + 32], F32)
    epsb = P.sb("epsb", [128, 1], F32)
    P.load("sp", cb, cb[:], cb_d[:, :])
    P.load("sp", cf, cf[:], cf_d[:, :])
    P.op("dve", lambda e: e.memset(epsb[:], EPS), w=[epsb])
    return dict(cb=cb, cf=cf, epsb=epsb, ident_b=cb[:, 0:128], U_b=cb[:, 128:256], ones_b=cb[:, 256:384],
                ident_f=cf[:, 0:128], ebase=cf[:, 128:160])


def host_consts():
    cb = np.zeros((128, 384), np.float32)
    cb[:, 0:128] = np.eye(128)
    cb[:, 128:256] = np.triu(np.ones((128, 128)), 1)
    cb[:, 256:384] = 1.0
    cf = np.zeros((128, 160), np.float32)
    cf[:, 0:128] = np.eye(128)
    cf[:, 128:160] = (np.arange(32) * CAP)[None, :]
    return cb.astype(NPBF), cf


def moe_dram_inputs(nc, pfx):
    W = {}
    W["g_bc"] = nc.dram_tensor(pfx + "g_bc", [128, D], F32, kind="ExternalInput")
    W["g_fm"] = nc.dram_tensor(pfx + "g_fm", [128, 8], F32, kind="ExternalInput")
    W["wr"] = nc.dram_tensor(pfx + "wr", [D, 36], F32, kind="ExternalInput").ap()
    W["br_bc"] = nc.dram_tensor(pfx + "br_bc", [128, 36], F32, kind="ExternalInput")
    W["w_gate"] = nc.dram_tensor(pfx + "w_gate", [E, D, 512], F32, kind="ExternalInput")
    W["w_up"] = nc.dram_tensor(pfx + "w_up", [E, D, 512], F32, kind="ExternalInput")
    W["w_down"] = nc.dram_tensor(pfx + "w_down", [E, 512, D], F32, kind="ExternalInput")
    return W


def moe_host_inputs(pfx, l, inp):
    g = inp["ffn_norm"][l]
    return {
        pfx + "g_bc": np.ascontiguousarray(np.broadcast_to(g[None, :], (128, D))),
        pfx + "g_fm": np.ascontiguousarray(g.reshape(8, 128).T),
        pfx + "wr": np.ascontiguousarray(np.concatenate([inp["router_group_w"][l], inp["router_expert_w"][l]], axis=1)),
        pfx + "br_bc": np.ascontiguousarray(np.broadcast_to(
            np.concatenate([inp["router_group_b"][l], inp["router_expert_b"][l]])[None, :], (128, 36))),
        pfx + "w_gate": inp["w_gate"][l],
        pfx + "w_up": inp["w_up"][l],
        pfx + "w_down": inp["w_down"][l],
    }


def declare_A(nc):
    io = {}
    io["xin"] = nc.dram_tensor("xin", [TPC + 128, D], F32, kind="ExternalInput")
    io["cb"] = nc.dram_tensor("cb", [128, 384], BF16, kind="ExternalInput")
    io["cf"] = nc.dram_tensor("cf", [128, 160], F32, kind="ExternalInput")
    io["pvec"] = nc.dram_tensor("pvec", [3, 128, D], F32, kind="ExternalInput")
    io["pool_w"] = nc.dram_tensor("pool_w", [4, 256, 256], F32, kind="ExternalInput")
    io["amain"] = nc.dram_tensor("amain", [128, 8, 128], BF16, kind="ExternalInput")
    io["ahalo"] = nc.dram_tensor("ahalo", [128, 8, 128], BF16, kind="ExternalInput")
    io["invc"] = nc.dram_tensor("invc", [128, 8, 128], F32, kind="ExternalInput")
    io["lvec"] = nc.dram_tensor("lvec", [2, 128, D], F32, kind="ExternalInput")
    io["lvec2"] = nc.dram_tensor("lvec2", [128, 640], F32, kind="ExternalInput")
    io["w_dkv"] = nc.dram_tensor("w_dkv", [D, 320], F32, kind="ExternalInput")
    io["wq_down"] = nc.dram_tensor("wq_down", [D, 384], F32, kind="ExternalInput")
    io["cs"] = nc.dram_tensor("cs", [128, NT, 64], F32, kind="ExternalInput")
    io["W0"] = moe_dram_inputs(nc, "m0_")
    io["x2"] = nc.dram_tensor("x2", [TPC, D], F32)
    io["latT"] = [nc.dram_tensor(f"latT{k}", [128, TPC], BF16) for k in range(6)]
    io["xs1"] = nc.dram_tensor("xs1", [TPC, D], F32)
    return io


def phase_A_pool(P, nc, PS, C, io):
    xin, pvec_d, poolw_d = io["xin"], io["pvec"], io["pool_w"]
    amain_d, ahalo_d, invc_d = io["amain"], io["ahalo"], io["invc"]
    xs1 = io["xs1"]
    epsb = C["epsb"]

    NXT = 4
    xt = [P.sb(f"pxt{s}", [128, D], F32) for s in range(NXT)]
    hb = [P.shared(f"hb{s}", [128, D], BF16) for s in range(2)]
    scr = P.shared("scr", [128, D], F32)
    ss2 = [P.sb(f"pss{s}", [128, 1], F32) for s in range(2)]
    rstd2 = [P.sb(f"prstd{s}", [128, 1], F32) for s in range(2)]
    pvec = [P.shared(f"vec{j}", [128, D], F32) for j in range(3)]
    pw = P.sb("pw", [128, 4, 2, 256], BF16)
    amain = P.sb("amain_s", [128, 8, 128], BF16)
    ahalo = P.sb("ahalo_s", [128, 8, 128], BF16)
    invc = P.sb("invc_s", [128, 8, 128], F32)
    pooledT2 = [P.sb(f"pooledT{s}", [128, 8, 128], BF16) for s in range(2)]
    tmpf2 = [P.sb(f"ptmpf{s}", [128, D], F32) for s in range(2)]
    xo1 = [P.shared(f"xo{s}", [128, D], F32) for s in range(2)]
    for j in range(3):
        P.load("sp", pvec[j], pvec[j][:], pvec_d[j, :, :])
    P.dma("pool", lambda e: e.dma_start(out=pw[:], in_=poolw_d.ap().rearrange("g (kk p) d -> p g kk d", p=128)),
          w=[pw], lane="pw")
    P.load("sp", amain, amain[:], amain_d[:, :, :])
    P.load("sp", ahalo, ahalo[:], ahalo_d[:, :, :])
    P.load("sp", invc, invc[:], invc_d[:, :, :])
    xs1_parts = [Tn(None, f"xs1p{i}") for i in range(NT)]

    def tile_state(i):
        s = (i + 1) % 2
        return dict(s=s, x_=xt[(i + 1) % NXT], h_=hb[s], hp=hb[1 - s], ss=ss2[s], rstd=rstd2[s], pooledT=pooledT2[s],
                    tmpf=tmpf2[s], bank=(PS[0:4] if s == 0 else PS[4:8]))

    def head(i):
        st = tile_state(i)
        x_, h_, hp, ss, rstd, bank = st["x_"], st["h_"], st["hp"], st["ss"], st["rstd"], st["bank"]
        emit_rstd(P, (x_[:], x_), scr, ss, rstd, epsb, D)
        P.op("dve", lambda e: e.scalar_tensor_tensor(out=h_[:], in0=x_[:], scalar=rstd[:, 0:1], in1=pvec[0][:],
                                                     op0=ALU.mult, op1=ALU.mult), r=[x_, rstd, pvec[0]], w=[h_])
        if i < 0:
            return
        v = 0 if i == 0 else 1
        for half, pp in ((0, bank[0]), (1, bank[1])):
            def pe(e, half=half, pp=pp):
                ins = None
                for kk in range(4):
                    k = half * 4 + kk
                    blk = v * 4 + k // 2
                    e.matmul(pp[:, kk * 128:(kk + 1) * 128], lhsT=h_[:, k * 128:(k + 1) * 128],
                             rhs=amain[:, blk, :], start=True, stop=False)
                    ins = e.matmul(pp[:, kk * 128:(kk + 1) * 128], lhsT=hp[64:128, k * 128:(k + 1) * 128],
                                   rhs=ahalo[64:128, blk, :], start=False, stop=True)
                return ins
            P.op("pe", pe, r=[h_, hp, amain, ahalo], w=[pp])

    def tail(i):
        st = tile_state(i)
        x_, pooledT, tmpf, bank = st["x_"], st["pooledT"], st["tmpf"], st["bank"]
        v = 0 if i == 0 else 1
        for half, pp in ((0, bank[0]), (1, bank[1])):
            def ev(e, half=half, pp=pp):
                ins = None
                for kk in range(4):
                    k = half * 4 + kk
                    blk = v * 4 + k // 2
                    ins = e.tensor_tensor(out=pooledT[:, k, :], in0=pp[:, kk * 128:(kk + 1) * 128],
                                          in1=invc[:, blk, :], op=ALU.mult)
                return ins
            P.op("dve", ev, r=[pp, invc], w=[pooledT])
        for half, py in ((0, bank[2]), (1, bank[3])):
            def pe(e, half=half, py=py):
                ins = None
                for gg in range(2):
                    g = half * 2 + gg
                    for kk in range(2):
                        ins = e.matmul(py[:, gg * 256:(gg + 1) * 256], lhsT=pooledT[:, 2 * g + kk, :],
                                       rhs=pw[:, g, kk, :], start=(kk == 0), stop=(kk == 1))
                return ins
            P.op("pe", pe, r=[pooledT, pw], w=[py])
            P.op("dve", lambda e, half=half, py=py: e.tensor_tensor(
                out=tmpf[:, half * 512:(half + 1) * 512], in0=py[:, :], in1=pvec[1][:, half * 512:(half + 1) * 512],
                op=ALU.add), r=[py, pvec[1], tmpf], w=[tmpf])
        xo_ = xo1[i % 2]
        P.op("pool", lambda e: e.tensor_tensor(out=tmpf[:], in0=tmpf[:], in1=pvec[2][:], op=ALU.mult),
             r=[tmpf, pvec[2]], w=[tmpf])
        P.op("pool", lambda e: e.tensor_tensor(out=xo_[:], in0=tmpf[:], in1=x_[:], op=ALU.add), r=[tmpf, x_], w=[xo_])
        P.store("sp", xs1[i * 128:(i + 1) * 128, :], xo_, xo_[:], w=[xs1_parts[i]])

    def load(i):
        x_ = xt[(i + 1) % NXT]
        P.load("sp", x_, x_[:], xin[(i + 1) * 128:(i + 2) * 128, :])

    AHEAD = NXT - 2
    for i in range(-1, min(NT, -1 + AHEAD + 1)):
        load(i)
    head(-1)
    head(0)
    for i in range(NT):
        if i + AHEAD < NT:
            load(i + AHEAD)
        if i + 1 < NT:
            head(i + 1)
        tail(i)
    io["xs1_parts"] = xs1_parts


def phase_A_moe(P, nc, PS, C, io):
    lvec_d, lvec2_d = io["lvec"], io["lvec2"]
    wdkv_d, wqd_d, cs_d, W, x2_d, xs1 = io["w_dkv"], io["wq_down"], io["cs"], io["W0"], io["x2"], io["xs1"]
    latT_d = io["latT"]
    xs1_parts = io["xs1_parts"]
    epsb = C["epsb"]
    lv = [P.shared(f"vec{j}", [128, D], F32) for j in range(2)]
    lv2 = P.sb("lv2", [128, 640], F32)
    wdkv = P.sb("wdkv", [128, 8, 320], BF16)
    wqd = P.sb("wqd", [128, 8, 384], BF16)
    cs = P.sb("cs_s", [128, NT, 64], F32)
    hk = P.sb("hk", [128, D], BF16)
    hq = P.sb("hq", [128, D], BF16)
    hkT = P.sb("hkT", [128, 8, 128], BF16)
    hqT = P.sb("hqT", [128, 8, 128], BF16)
    lat = P.sb("lat", [128, 768], BF16)
    latT = P.sb("latT_s", [128, 6, TPC], BF16)
    ss2 = P.sb("lss2", [128, 1], F32)
    rs2 = P.sb("lrs2", [128, 1], F32)
    ss3 = P.sb("lss3", [128, 1], F32)
    rs3 = P.sb("lrs3", [128, 1], F32)
    ssl = P.sb("lssl", [128, 1], F32)
    rsl = P.sb("lrsl", [128, 1], F32)
    lscr = P.shared("scr", [128, D], F32)
    rt = P.sb("rt", [128, 4, 32], F32)
    for j in range(2):
        P.load("sp", lv[j], lv[j][:], lvec_d[j, :, :])
    P.load("sp", lv2, lv2[:], lvec2_d[:, :])
    P.load("sp", cs, cs[:], cs_d[:, :, :])
    P.dma("pool", lambda e: e.dma_start(out=wdkv[:], in_=wdkv_d.ap().rearrange("(k p) f -> p k f", p=128)),
          w=[wdkv], lane="wdkv")
    P.dma("pool", lambda e: e.dma_start(out=wqd[:], in_=wqd_d.ap().rearrange("(k p) f -> p k f", p=128)),
          w=[wqd], lane="wqd")
    P.op("pool", lambda e: e.memset(lat[:, 704:768], 0.0), w=[lat])

    x2_parts = [Tn(None, f"x2p{i}") for i in range(NT)]
    io["x2_parts"] = x2_parts

    def post(i, xo_):
        P.store("sp", x2_d[i * 128:(i + 1) * 128, :], xo_, xo_[:], w=[x2_parts[i]])
        emit_rstd(P, (xo_[:], xo_), lscr, ssl, rsl, epsb, D)
        P.op("dve", lambda e: e.scalar_tensor_tensor(out=hk[:], in0=xo_[:], scalar=rsl[:, 0:1], in1=lv[0][:],
                                                     op0=ALU.mult, op1=ALU.mult), r=[xo_, rsl, lv[0]], w=[hk])
        P.op("dve", lambda e: e.scalar_tensor_tensor(out=hq[:], in0=xo_[:], scalar=rsl[:, 0:1], in1=lv[1][:],
                                                      op0=ALU.mult, op1=ALU.mult), r=[xo_, rsl, lv[1]], w=[hq])
        emit_transposes(P, hk, hkT, PS[5], C["ident_b"], C["cb"], 8, evac="act")
        emit_transposes(P, hq, hqT, PS[6], C["ident_b"], C["cb"], 8, evac="dve")
        pkv, pq = PS[7], PS[5]

        def pe1(e):
            ins = None
            for k in range(8):
                ins = e.matmul(pkv[:, 0:320], lhsT=hkT[:, k, :], rhs=wdkv[:, k, :], start=(k == 0), stop=(k == 7))
            return ins
        P.op("pe", pe1, r=[hkT, wdkv], w=[pkv])

        def pe2(e):
            ins = None
            for k in range(8):
                ins = e.matmul(pq[:, 0:384], lhsT=hqT[:, k, :], rhs=wqd[:, k, :], start=(k == 0), stop=(k == 7))
            return ins
        P.op("pe", pe2, r=[hqT, wqd], w=[pq])
        emit_rstd(P, (pkv[:, 0:256], pkv), lscr, ss2, rs2, epsb, 256)
        P.op("dve", lambda e: e.scalar_tensor_tensor(out=lat[:, 384:640], in0=pkv[:, 0:256], scalar=rs2[:, 0:1],
                                                     in1=lv2[:, 0:256], op0=ALU.mult, op1=ALU.mult),
             r=[pkv, rs2, lv2, lat], w=[lat])
        cosv = cs[:, i, 0:32]
        sinv = cs[:, i, 32:64]

        def rope(e):
            e.tensor_tensor(out=rt[:, 0, :], in0=pkv[:, 256:288], in1=cosv, op=ALU.mult)
            e.tensor_tensor(out=rt[:, 1, :], in0=pkv[:, 288:320], in1=sinv, op=ALU.mult)
            e.tensor_tensor(out=rt[:, 2, :], in0=pkv[:, 288:320], in1=cosv, op=ALU.mult)
            return e.tensor_tensor(out=rt[:, 3, :], in0=pkv[:, 256:288], in1=sinv, op=ALU.mult)
        P.op("dve", rope, r=[pkv, cs], w=[rt])

        def rope2(e):
            e.tensor_tensor(out=lat[:, 640:672], in0=rt[:, 0, :], in1=rt[:, 1, :], op=ALU.subtract)
            return e.tensor_tensor(out=lat[:, 672:704], in0=rt[:, 2, :], in1=rt[:, 3, :], op=ALU.add)
        P.op("dve", rope2, r=[rt, lat], w=[lat])
        emit_rstd(P, (pq[:, 0:384], pq), lscr, ss3, rs3, epsb, 384)
        P.op("dve", lambda e: e.scalar_tensor_tensor(out=lat[:, 0:384], in0=pq[:, 0:384], scalar=rs3[:, 0:1],
                                                     in1=lv2[:, 256:640], op0=ALU.mult, op1=ALU.mult),
             r=[pq, rs3, lv2, lat], w=[lat])
        emit_transposes(P, lat, latT, PS[6], C["ident_b"], C["cb"], 6, evac="act", dst_off=i * 128)

    moe_layer(P, "m0_", xs1, xs1_parts, PS, C, W, post)
    io["latT_done"] = [Tn(None, f"latT_done{k}") for k in range(6)]
    for k in range(6):
        P.dma("sp", lambda e, k=k: e.dma_start(out=latT_d[k][:, :], in_=latT[:, k, :]), r=[latT],
              w=[io["latT_done"][k]], lane=f"latT_out{k}")


def pool_consts(core):
    seq_start = (core % 4 == 0)
    wins = (2, 4, 8, 16)
    amain = np.zeros((128, 8, 128), np.float32)
    ahalo = np.zeros((128, 8, 128), np.float32)
    invc = np.zeros((128, 8, 128), np.float32)
    for v in range(2):
        for g, w in enumerate(wins):
            t = np.arange(128)
            if v == 0 and seq_start:
                cntv = np.minimum(t + 1, w).astype(np.float32)
            else:
                cntv = np.full(128, w, np.float32)
            B = np.zeros((128 + 64, 128), np.float32)
            for tt in range(128):
                lo = tt - w + 1
                B[lo + 64:tt + 64 + 1, tt] = 1.0
                B[tt + 64, tt] -= cntv[tt]
            amain[:, v * 4 + g, :] = B[64:, :]
            ahalo[64:, v * 4 + g, :] = B[:64, :]
            invc[:, v * 4 + g, :] = (1.0 / cntv)[None, :]
    return amain.astype(NPBF), ahalo.astype(NPBF), invc


def rope_np():
    half = 32
    inv = (np.float32(10000.0) ** (-np.arange(half, dtype=np.float32) / np.float32(half))).astype(np.float32)
    ang = np.arange(SEQ, dtype=np.float32)[:, None] * inv[None, :]
    return np.cos(ang).astype(np.float32), np.sin(ang).astype(np.float32)


_CACHE = {}

NQT = SEQ // 512


def declare_B(nc):
    io = {}
    io["wqn"] = nc.dram_tensor("wqn", [2, 384, 128], F32, kind="ExternalInput")
    io["wqr"] = nc.dram_tensor("wqr", [2, 384, 64], F32, kind="ExternalInput")
    io["wuk"] = nc.dram_tensor("wuk", [2, 256, 128], F32, kind="ExternalInput")
    io["wuv"] = nc.dram_tensor("wuv", [2, 256, 128], F32, kind="ExternalInput")
    io["csT"] = nc.dram_tensor("csT", [64, 2, SEQ], F32, kind="ExternalInput")
    io["mask"] = nc.dram_tensor("mask", [128, 4, 512], BF16, kind="ExternalInput")
    io["G"] = [nc.dram_tensor(f"latG{k}", [4 * 128, TPC], BF16) for k in range(6)]
    io["OTx"] = [nc.dram_tensor(f"OTx{q}", [128, TPC], BF16) for q in range(8)]
    io["OTg"] = nc.dram_tensor("OTall", [8 * 4 * 128, TPC], BF16)
    io["otidx"] = nc.dram_tensor("otidx", [128, 8], I32, kind="ExternalInput")
    return io


def phase_B(P, nc, PS, C, io):
    wqn_d, wqr_d, wuk_d, wuv_d, cs_d, mask_d = io["wqn"], io["wqr"], io["wuk"], io["wuv"], io["csT"], io["mask"]
    Gv = [g_.ap().rearrange("(r p) t -> r p t", r=4) for g_ in io["G"]]
    OTx = io["OTx"]
    Gdone = io["G_done"]
    ckvT = P.sb("ckvT", [128, 2, SEQ], BF16)
    krT = P.sb("krT", [128, SEQ], BF16)
    KT = P.sb("KT", [128, SEQ], BF16)
    V = P.sb("V", [128, 64, 128], BF16)
    cqt = [P.sb(f"cqt{s}", [128, 3, 512], BF16) for s in range(2)]
    cst = [P.sb(f"cst{s}", [128, 2, 512], F32) for s in range(2)]
    qn = [P.sb(f"qn{s}", [128, 512], BF16) for s in range(2)]
    qr = [P.sb(f"qr{s}", [128, 512], BF16) for s in range(2)]
    PT = [P.sb(f"PT{s}", [128, 512], BF16) for s in range(3)]
    mask = P.sb("mask_s", [128, 4, 512], BF16)
    t1 = P.sb("t1", [128, 512], F32)
    t2 = P.sb("t2", [128, 512], F32)
    rden = P.sb("rden", [128, 512], F32)
    oo = [P.sb(f"oo{s}", [128, 512], BF16) for s in range(2)]
    wqn = P.sb("wqn_s", [128, 2, 3, 128], BF16)
    wqr = P.sb("wqr_s", [128, 2, 3, 64], BF16)
    wqt = P.sb("wqt_s", [128, 2, 3, 64], BF16)
    wuk = P.sb("wuk_s", [128, 2, 2, 128], BF16)
    wuv = P.sb("wuv_s", [128, 2, 2, 128], BF16)

    for r_ in range(4):
        for c_ in range(2):
            P.load("sp", ckvT, ckvT[:, c_, r_ * TPC:(r_ + 1) * TPC], Gv[3 + c_][r_, :, :], r=[Gdone[3 + c_]])
        P.load("sp", krT, krT[0:64, r_ * TPC:(r_ + 1) * TPC], Gv[5][r_, 0:64, :], r=[Gdone[5]])
    P.load("sp", mask, mask[:], mask_d[:, :, :])
    ones_ap, ones_buf = C["ones_b"], C["cb"]
    for (t, d) in ((wqn, wqn_d), (wqr, wqr_d), (wuk, wuk_d), (wuv, wuv_d)):
        P.dma("pool", lambda e, t=t, d=d: e.dma_start(out=t[:], in_=d.ap().rearrange("h (k p) f -> p h k f", p=128)),
              w=[t], lane=t.name)
    P.op("dve", lambda e: e.tensor_scalar(out=wqt[:, :, :, 0:32], in0=wqr[:, :, :, 32:64], scalar1=-1.0, scalar2=None,
                                          op0=ALU.mult), r=[wqr], w=[wqt])
    P.op("dve", lambda e: e.tensor_copy(out=wqt[:, :, :, 32:64], in_=wqr[:, :, :, 0:32]), r=[wqr, wqt], w=[wqt])

    pS = PS[0:3]
    pO, pD, pQ, pQr, pQt = PS[3], PS[4], PS[5], PS[6], PS[7]
    pending_o = []
    pending_cp = []
    tile_no = 0

    def load_q_inputs(qi):
        cq_, cs_ = cqt[qi % 2], cst[qi % 2]
        for c_ in range(3):
            P.load("sp", cq_, cq_[:, c_, :], Gv[c_][qi // 4, :, (qi % 4) * 512:(qi % 4 + 1) * 512], r=[Gdone[c_]])
        P.load("sp", cs_, cs_[0:64, :, :], cs_d[:, :, qi * 512:(qi + 1) * 512])

    def emit_qproj(h, qi):
        cq_, cs_, qn_, qr_ = cqt[qi % 2], cst[qi % 2], qn[qi % 2], qr[qi % 2]

        def pe_q(e):
            for c in range(3):
                e.matmul(pQ[:, :], lhsT=wqn[:, h, c, :], rhs=cq_[:, c, :], start=(c == 0), stop=(c == 2))
            for c in range(3):
                e.matmul(pQr[0:64, :], lhsT=wqr[:, h, c, :], rhs=cq_[:, c, :], start=(c == 0), stop=(c == 2))
            ins = None
            for c in range(3):
                ins = e.matmul(pQt[0:64, :], lhsT=wqt[:, h, c, :], rhs=cq_[:, c, :], start=(c == 0), stop=(c == 2))
            return ins
        P.op("pe", pe_q, r=[wqn, wqr, wqt, cq_], w=[pQ, pQr, pQt])
        P.op("act", lambda e: e.mul(out=qn_[:], in_=pQ[:, :], mul=ATTN_SCALE), r=[pQ], w=[qn_])
        P.op("dve", lambda e: e.scalar_tensor_tensor(out=t1[0:64, :], in0=pQr[0:64, :], scalar=ATTN_SCALE,
                                                     in1=cs_[0:64, 0, :], op0=ALU.mult, op1=ALU.mult),
             r=[pQr, cs_], w=[t1])
        P.op("dve", lambda e: e.scalar_tensor_tensor(out=t2[0:64, :], in0=pQt[0:64, :], scalar=ATTN_SCALE,
                                                     in1=cs_[0:64, 1, :], op0=ALU.mult, op1=ALU.mult),
             r=[pQt, cs_], w=[t2])
        P.op("dve", lambda e: e.tensor_tensor(out=qr_[0:64, :], in0=t1[0:64, :], in1=t2[0:64, :], op=ALU.add),
             r=[t1, t2], w=[qr_])

    for h in range(2):
        for kt16 in range(16):
            pk = PS[5 + (kt16 % 2)]

            def pe(e, kt16=kt16, pk=pk, h=h):
                ins = None
                for c in range(2):
                    ins = e.matmul(pk[:, :], lhsT=wuk[:, h, c, :], rhs=ckvT[:, c, kt16 * 512:(kt16 + 1) * 512],
                                   start=(c == 0), stop=(c == 1))
                return ins
            P.op("pe", pe, r=[wuk, ckvT], w=[pk])
            if kt16 % 2 == 0:
                P.op("act", lambda e, kt16=kt16, pk=pk: e.copy(out=KT[:, kt16 * 512:(kt16 + 1) * 512], in_=pk[:, :]),
                     r=[pk], w=[KT])
            else:
                P.op("dve", lambda e, kt16=kt16, pk=pk: e.tensor_copy(out=KT[:, kt16 * 512:(kt16 + 1) * 512], in_=pk[:, :]),
                     r=[pk], w=[KT])
        for kg in range(16):
            pv = PS[5 + (kg % 2)]

            def pe(e, kg=kg, pv=pv, h=h):
                ins = None
                for k4 in range(4):
                    kt = kg * 4 + k4
                    for c in range(2):
                        ins = e.matmul(pv[:, k4 * 128:(k4 + 1) * 128], lhsT=ckvT[:, c, kt * 128:(kt + 1) * 128],
                                       rhs=wuv[:, h, c, :], start=(c == 0), stop=(c == 1))
                return ins
            P.op("pe", pe, r=[wuv, ckvT], w=[pv])
            if kg % 2 == 0:
                P.op("act", lambda e, kg=kg, pv=pv: e.copy(out=V[:, kg * 4:(kg + 1) * 4, :],
                                                           in_=pv[:, :].rearrange("p (k d) -> p k d", k=4)), r=[pv], w=[V])
            else:
                P.op("dve", lambda e, kg=kg, pv=pv: e.tensor_copy(out=V[:, kg * 4:(kg + 1) * 4, :],
                                                                  in_=pv[:, :].rearrange("p (k d) -> p k d", k=4)),
                     r=[pv], w=[V])
        for qi in range(NQT):
            while pending_cp and pending_cp[0][0] < tile_no:
                io["emit_o_copy"](pending_cp.pop(0)[1])
            while pending_o and pending_o[0][0] < tile_no:
                q_ = pending_o.pop(0)[1]
                io["emit_o_cc"](q_)
                pending_cp.append((tile_no, q_))
            tile_no += 1
            s2 = qi % 2
            cq_, cs_, qn_, qr_ = cqt[s2], cst[s2], qn[s2], qr[s2]
            if qi == 0:
                load_q_inputs(0)
                load_q_inputs(1)

            if qi == 0:
                emit_qproj(h, 0)
                load_q_inputs(2)
            nk = 4 * (qi + 1)

            def emit_S(kt, qn_=qn_, qr_=qr_):
                ps = pS[kt % 3]

                def pe(e, kt=kt, ps=ps):
                    e.matmul(ps[:, :], lhsT=KT[:, kt * 128:(kt + 1) * 128], rhs=qn_[:, :], start=True, stop=False)
                    return e.matmul(ps[:, :], lhsT=krT[0:64, kt * 128:(kt + 1) * 128], rhs=qr_[0:64, :],
                                    start=False, stop=True)
                P.op("pe", pe, r=[KT, krT, qn_, qr_], w=[ps])

            emit_S(0)
            for kt in range(nk):
                if kt == 0 and qi + 1 < NQT:
                    emit_qproj(h, qi + 1)
                    if qi + 3 < NQT:
                        load_q_inputs(qi + 3)
                if kt + 1 < nk:
                    emit_S(kt + 1)
                ps = pS[kt % 3]
                pt = PT[kt % 3]
                P.op("act", lambda e, ps=ps, pt=pt: e.activation(out=pt[:], in_=ps[:, :], func=AF.Exp), r=[ps], w=[pt])
                if kt >= 4 * qi:
                    j = kt - 4 * qi
                    P.op("dve", lambda e, pt=pt, j=j: e.tensor_tensor(out=pt[:], in0=pt[:], in1=mask[:, j, :], op=ALU.mult),
                         r=[pt, mask], w=[pt])

                def pe_pv(e, kt=kt, pt=pt, nk=nk):
                    e.matmul(pO[:, :], lhsT=V[:, kt, :], rhs=pt[:], start=(kt == 0), stop=(kt == nk - 1))
                    return e.matmul(pD[:, :], lhsT=ones_ap, rhs=pt[:], start=(kt == 0), stop=(kt == nk - 1))
                P.op("pe", pe_pv, r=[V, pt, ones_buf], w=[pO, pD])
            P.op("dve", lambda e: e.reciprocal(out=rden[:], in_=pD[:, :]), r=[pD], w=[rden])
            oo_ = oo[s2]
            P.op("dve", lambda e, oo_=oo_: e.tensor_tensor(out=oo_[:], in0=pO[:, :], in1=rden[:], op=ALU.mult),
                 r=[pO, rden], w=[oo_])
            P.store("sp", OTx[(qi // 4) * 2 + h][:, (qi % 4) * 512:(qi % 4 + 1) * 512], oo_, oo_[:],
                    w=[io["OTx_parts"][h * NQT + qi]])
            if qi % 4 == 3:
                pending_o.append((tile_no, (qi // 4) * 2 + h))
    while pending_cp:
        io["emit_o_copy"](pending_cp.pop(0)[1])
    while pending_o:
        q_ = pending_o.pop(0)[1]
        io["emit_o_cc"](q_)
        io["emit_o_copy"](q_)


def attn_mask_np():
    m = np.zeros((128, 4, 512), np.float32)
    for j in range(4):
        kc = (128 * j + np.arange(128)) // 64
        qc = np.arange(512) // 64
        m[:, j, :] = (kc[:, None] <= qc[None, :]).astype(np.float32)
    return m.astype(NPBF)


def declare_C(nc):
    io = {}
    io["wo"] = nc.dram_tensor("wo", [D, D], F32, kind="ExternalInput")
    io["fg"] = nc.dram_tensor("fg", [128, D], F32, kind="ExternalInput")
    io["W1"] = moe_dram_inputs(nc, "m1_")
    io["out"] = nc.dram_tensor("out", [TPC, D], F32, kind="ExternalOutput")
    io["xs3"] = nc.dram_tensor("xs3", [TPC, D], F32)
    return io


def phase_C(P, nc, PS, C, io):
    x2_d, wo_d, fg_d, W, out_d, xs3 = io["x2"], io["wo"], io["fg"], io["W1"], io["out"], io["xs3"]
    ot_d = io["OTg"]
    epsb = C["epsb"]
    xt = [P.shared(f"xt{s}", [128, D], F32) for s in range(2)]
    xo = [P.shared(f"xo{s}", [128, D], F32) for s in range(2)]
    scr = P.shared("scr", [128, D], F32)
    OTs = P.sb("OTs", [128, 8, TPC], BF16)
    wo = P.sb("wo_s", [128, 8, D], BF16)
    fg = P.sb("fg_s", [128, D], F32)
    fss = P.sb("fss", [128, 1], F32)
    frs = P.sb("frs", [128, 1], F32)
    fo = [P.sb(f"fo{s}", [128, D], F32) for s in range(2)]
    otidx = P.sb("otidx_s", [128, 8], I32)
    P.load("sp", otidx, otidx[:], io["otidx"][:, :])
    for k in range(8):
        P.dma("pool", lambda e, k=k: e.indirect_dma_start(
            out=OTs[:, k, :], out_offset=None, in_=ot_d[:, :],
            in_offset=bass.IndirectOffsetOnAxis(ap=otidx[:, k:k + 1], axis=0)),
            r=list(io["OTg_done"]) + [otidx], w=[OTs], lane="otg_gather")
    P.load("sp", fg, fg[:], fg_d[:, :])
    P.dma("pool", lambda e: e.dma_start(out=wo[:], in_=wo_d.ap().rearrange("(k p) f -> p k f", p=128)), w=[wo], lane="wo")
    xs3_parts = [Tn(None, f"xs3p{i}") for i in range(NT)]
    P.load("sp", xt[0], xt[0][:], x2_d[0:128, :], r=io["x2_parts"])
    for i in range(NT):
        s = i % 2
        x_, xo_ = xt[s], xo[s]
        if i + 1 < NT:
            xn = xt[(i + 1) % 2]
            P.load("sp", xn, xn[:], x2_d[(i + 1) * 128:(i + 2) * 128, :], r=io["x2_parts"])
        for dh in range(2):
            pa = PS[2 * (i % 2) + dh]

            def pe(e, dh=dh, pa=pa, i=i):
                ins = None
                for k in range(8):
                    ins = e.matmul(pa[:, :], lhsT=OTs[:, k, i * 128:(i + 1) * 128], rhs=wo[:, k, dh * 512:(dh + 1) * 512],
                                   start=(k == 0), stop=(k == 7))
                return ins
            P.op("pe", pe, r=[OTs, wo], w=[pa])
            P.op("dve", lambda e, dh=dh, pa=pa, x_=x_, xo_=xo_: e.tensor_tensor(
                out=xo_[:, dh * 512:(dh + 1) * 512], in0=pa[:, :], in1=x_[:, dh * 512:(dh + 1) * 512], op=ALU.add),
                r=[pa, x_, xo_], w=[xo_])
        P.store("sp", xs3[i * 128:(i + 1) * 128, :], xo_, xo_[:], w=[xs3_parts[i]])

    def post(i, xo_):
        emit_rstd(P, (xo_[:], xo_), scr, fss, frs, epsb, D)
        fo_ = fo[i % 2]
        P.op("dve", lambda e: e.scalar_tensor_tensor(out=fo_[:], in0=xo_[:], scalar=frs[:, 0:1], in1=fg[:],
                                                     op0=ALU.mult, op1=ALU.mult), r=[xo_, frs, fg], w=[fo_])
        P.store("sp", out_d[i * 128:(i + 1) * 128, :], fo_, fo_[:], final=True)

    moe_layer(P, "m1_", xs3, xs3_parts, PS, C, W, post)


GROUPS = [[0, 1, 2, 3], [4, 5, 6, 7]]


def build_F():
    nc = bass.Bass("TRN2", target_bir_lowering=False)
    P = Prog(nc)
    io = declare_A(nc)
    io.update(declare_B(nc))
    io.update(declare_C(nc))
    PS = [P.psum(f"ps{b}", [128, 512], F32) for b in range(8)]
    C = load_consts(P, io["cb"], io["cf"])
    mark = P.mark()
    phase_A_pool(P, nc, PS, C, io)
    P.release(mark)
    phase_A_moe(P, nc, PS, C, io)
    io["G_done"] = [Tn(None, f"G_done{k}") for k in range(6)]
    for k in range(6):
        P.dma("pool", lambda e, k=k: e.collective_compute("AllGather", ALU.bypass, replica_groups=GROUPS,
                                                          ins=[io["latT"][k].ap().opt()], outs=[io["G"][k].ap().opt()]),
              r=[io["latT_done"][k]], w=[io["G_done"][k]], lane=f"cc_ag{k}", inc=1)
    P.release(mark)
    io["OTx_parts"] = [Tn(None, f"otx{i}") for i in range(2 * NQT)]
    io["OTg_done"] = [Tn(None, f"OTg_done{q}") for q in range(8)]

    def emit_o_cc(q):
        j_, hh_ = q // 2, q % 2
        parts = [io["OTx_parts"][hh_ * NQT + qi] for qi in range(4 * j_, 4 * j_ + 4)]
        P.dma("pool", lambda e, q=q: e.collective_compute("AllGather", ALU.bypass, replica_groups=GROUPS,
                                                          ins=[io["OTx"][q].ap().opt()],
                                                          outs=[io["OTg"][q * 512:(q + 1) * 512, :].opt()]),
              r=parts, w=[io["OTg_done"][q]], lane=f"cc_o{q}", inc=1)

    def emit_o_copy(q):
        pass

    io["emit_o_cc"] = emit_o_cc
    io["emit_o_copy"] = emit_o_copy
    phase_B(P, nc, PS, C, io)
    P.release(mark)
    phase_C(P, nc, PS, C, io)
    P.finish()
    return nc


def kernel(**inputs):
    inp = {k: np.asarray(v) for k, v in inputs.items()}
    if "F" not in _CACHE:
        _CACHE["F"] = build_F()
    nc = _CACHE["F"]
    x = inp["x"].reshape(NCORES, TPC, D)
    cb, cf = host_consts()
    cosf, sinf = rope_np()
    bc = lambda v: np.ascontiguousarray(np.broadcast_to(v[None, :], (128, v.shape[0])))
    pvec = np.stack([bc(inp["pool_norm"][0]), bc(inp["pool_b"][0].reshape(-1)), bc(inp["pool_scale"][0])])
    lvec = np.stack([bc(inp["kv_in_norm"]), bc(inp["attn_norm"][0])])
    lvec2 = np.ascontiguousarray(np.concatenate([bc(inp["kv_norm"]), bc(inp["q_norm"][0])], axis=1))
    csT = np.ascontiguousarray(np.stack([np.concatenate([cosf.T, cosf.T], axis=0),
                                         np.concatenate([sinf.T, sinf.T], axis=0)], axis=1))
    wq = inp["wq_up"][0].reshape(384, 8, 192)
    wuk = inp["w_uk"].reshape(256, 8, 128)
    wuv = inp["w_uv"].reshape(256, 8, 128)
    common = {
        "cb": cb, "cf": cf, "pvec": pvec, "pool_w": inp["pool_w"][0], "lvec": lvec, "lvec2": lvec2,
        "w_dkv": inp["w_dkv"], "wq_down": inp["wq_down"][0], "csT": csT, "mask": attn_mask_np(),
        "wo": inp["wo"][0], "fg": bc(inp["final_norm"]),
    }
    common.update(moe_host_inputs("m0_", 0, inp))
    common.update(moe_host_inputs("m1_", 1, inp))
    in_maps = []
    for c in range(NCORES):
        m_ = c % 4
        halo = np.zeros((128, D), np.float32) if m_ == 0 else x[c - 1, -128:, :]
        am, ah, ic = pool_consts(c)
        p0 = m_ * TPC
        cs = np.concatenate([cosf[p0:p0 + TPC], sinf[p0:p0 + TPC]], axis=1)
        cs = np.ascontiguousarray(cs.reshape(NT, 128, 64).transpose(1, 0, 2))
        hs = [2 * m_, 2 * m_ + 1]
        m = dict(common)
        m.update({
            "xin": np.ascontiguousarray(np.concatenate([halo, x[c]], axis=0)), "amain": am, "ahalo": ah, "invc": ic, "cs": cs,
            "wqn": np.ascontiguousarray(np.stack([wq[:, h, 0:128] for h in hs])),
            "wqr": np.ascontiguousarray(np.stack([wq[:, h, 128:192] for h in hs])),
            "wuk": np.ascontiguousarray(np.stack([wuk[:, h, :] for h in hs])),
            "wuv": np.ascontiguousarray(np.stack([wuv[:, h, :] for h in hs])),
            "otidx": np.ascontiguousarray(np.stack(
                [(m_ * 2 + (k % 2)) * 512 + (k // 2) * 128 + np.arange(128) for k in range(8)], axis=1).astype(np.int32)),
        })
        in_maps.append(m)
    res = run_bass_kernel_spmd(nc, in_maps, core_ids=list(range(NCORES)))
    out = np.stack([np.asarray(r["out"]) for r in res.results])
    return out.reshape(2, SEQ, D).astype(np.float32)
```
